# Optimizing a Trainium2 kernel written in Bass

```python
import jax, jax.numpy as jnp
from jax import lax
import numpy as np

D_MODEL = 1024
BATCH = 8
SEQ = 4096
DEPTH = 1

GRID_W = 64
CTX_LEN = 256

GLA_HEADS = 4
GLA_DK = 64
GLA_DV = 128
GLA_GATE_RANK = 16
GLA_GATE_NORM = 16.0
GLA_CHUNK = 64
GLA_WIDTH = GLA_HEADS * GLA_DV

MLA_HEADS = 8
MLA_NOPE = 64
MLA_ROPE = 32
MLA_DV = 64
MLA_Q_RANK = 256
MLA_KV_RANK = 128
MLA_WIDTH = MLA_HEADS * MLA_DV
Q_BLOCK = 128
ROPE_BASE = 10000.0
ROPE_AXIS_PAIRS = MLA_ROPE // 4

MIX_WIDTH = GLA_WIDTH + MLA_WIDTH
IN_SPLITS = (GLA_HEADS * GLA_DK,
             GLA_HEADS * GLA_DK,
             GLA_WIDTH,
             GLA_GATE_RANK,
             GLA_GATE_RANK,
             GLA_WIDTH,
             MLA_Q_RANK,
             MLA_KV_RANK,
             MLA_ROPE)
IN_WIDTH = 1984

N_GROUPS = 4
EXPERTS_PER_GROUP = 4
N_EXPERTS = N_GROUPS * EXPERTS_PER_GROUP
TOP_K = 2
D_EXPERT = 256

DEEPNORM_ALPHA = (2.0 * DEPTH) ** 0.25
DEEPNORM_BETA = (8.0 * DEPTH) ** -0.25
EPS = 1e-6

kernel_name = "hymba_gla_mla_hmoe_dit_block"


def layernorm(x, g, b):
    xf = x.astype(jnp.float32)
    mu = xf.mean(-1, keepdims=True)
    var = jnp.square(xf - mu).mean(-1, keepdims=True)
    return ((xf - mu) * lax.rsqrt(var + EPS)).astype(x.dtype) * g + b


def rmsnorm(x, g):
    xf = x.astype(jnp.float32)
    return (xf * lax.rsqrt(jnp.square(xf).mean(-1, keepdims=True) + EPS)).astype(x.dtype) * g


def modulate(x, shift, scale):
    return x * (1.0 + scale) + shift


def rope_half(x, ang):
    h = x.shape[-1] // 2
    cos = jnp.cos(ang).astype(x.dtype)
    sin = jnp.sin(ang).astype(x.dtype)
    x1, x2 = x[..., :h], x[..., h:]
    return jnp.concatenate([x1 * cos - x2 * sin, x2 * cos + x1 * sin], axis=-1)


def rope2d(x, ang_row, ang_col):
    half = x.shape[-1] // 2
    return jnp.concatenate([rope_half(x[..., :half], ang_row),
                            rope_half(x[..., half:], ang_col)], axis=-1)


def gla_chunked(q, k, v, log_g, s0):
    B, T, H, DK = q.shape
    DV = v.shape[-1]
    N = T // GLA_CHUNK

    def to_chunks(a):
        return a.astype(jnp.float32).reshape(B, N, GLA_CHUNK, H, a.shape[-1]).transpose(0, 3, 1, 2, 4)

    qc = to_chunks(q) * (DK ** -0.5)
    kc, vc, gc = to_chunks(k), to_chunks(v), to_chunks(log_g)
    b = jnp.cumsum(gc, axis=3)
    b_last = b[:, :, :, -1:, :]
    q_e = qc * jnp.exp(b)
    k_e = kc * jnp.exp(-b)
    mask = jnp.tril(jnp.ones((GLA_CHUNK, GLA_CHUNK), dtype=bool))
    a = jnp.where(mask, jnp.einsum('bhnid,bhnjd->bhnij', q_e, k_e), 0.0)
    o_intra = jnp.einsum('bhnij,bhnjv->bhniv', a, vc)
    ds = jnp.einsum('bhncd,bhncv->bhndv', kc * jnp.exp(b_last - b), vc)
    decay = jnp.exp(b_last[:, :, :, 0, :])

    def step(s, inp):
        d_n, ds_n = inp
        return d_n[..., None] * s + ds_n, s

    s_final, s_prev = lax.scan(step, s0.astype(jnp.float32),
                               (jnp.moveaxis(decay, 2, 0), jnp.moveaxis(ds, 2, 0)))
    o_inter = jnp.einsum('bhncd,nbhdv->bhncv', q_e, s_prev)
    o = (o_intra + o_inter).transpose(0, 2, 3, 1, 4).reshape(B, T, H, DV)
    return o.astype(v.dtype), s_final


def gla_group(p_lat, p_ctx, w_gk_f, b_gk_f, w_gk_b, b_gk_b, gla_norm_g, need_ctx_out):
    def heads(p):
        q, k, v, gf, gb, r = p
        B, T, _ = q.shape
        sh = lambda a, d: a.reshape(B, T, GLA_HEADS, d)
        lg_f = jax.nn.log_sigmoid((gf @ w_gk_f + b_gk_f).astype(jnp.float32)) / GLA_GATE_NORM
        lg_b = jax.nn.log_sigmoid((gb @ w_gk_b + b_gk_b).astype(jnp.float32)) / GLA_GATE_NORM
        return sh(q, GLA_DK), sh(k, GLA_DK), sh(v, GLA_DV), sh(lg_f, GLA_DK), sh(lg_b, GLA_DK), sh(r, GLA_DV)

    def flip(a):
        return jnp.flip(a, axis=1)

    def out(o, r):
        B, T = o.shape[:2]
        return (rmsnorm(o, gla_norm_g) * jax.nn.silu(r)).reshape(B, T, GLA_WIDTH)

    q, k, v, lgf, lgb, r = heads(p_lat)
    qc, kc, vc, lgf_c, lgb_c, rc = heads(p_ctx)
    zeros = jnp.zeros((q.shape[0], GLA_HEADS, GLA_DK, GLA_DV), jnp.float32)
    o_cf, s_f = gla_chunked(qc, kc, vc, lgf_c, zeros)
    o_cb, s_b = gla_chunked(flip(qc), flip(kc), flip(vc), flip(lgb_c), zeros)
    o_f, _ = gla_chunked(q, k, v, lgf, s_f)
    o_b, _ = gla_chunked(flip(q), flip(k), flip(v), flip(lgb), s_b)
    lat = out(o_f + flip(o_b), r)
    ctx_out = out(o_cf + flip(o_cb), rc) if need_ctx_out else None
    return lat, ctx_out


def mla_group(p_lat, p_ctx, ang_row, ang_col, mla_q_norm_g, w_uq, mla_kv_norm_g, w_ukv, need_ctx_out):
    scale = (MLA_NOPE + MLA_ROPE) ** -0.5

    def project(p, rotate):
        cq, ckv, kr = p
        B, T, _ = cq.shape
        q = (rmsnorm(cq, mla_q_norm_g) @ w_uq).reshape(B, T, MLA_HEADS, MLA_NOPE + MLA_ROPE)
        qn, qr = q[..., :MLA_NOPE], q[..., MLA_NOPE:]
        kv = (rmsnorm(ckv, mla_kv_norm_g) @ w_ukv).reshape(B, T, MLA_HEADS, MLA_NOPE + MLA_DV)
        kn, v = kv[..., :MLA_NOPE], kv[..., MLA_NOPE:]
        if rotate:
            qr = rope2d(qr, ang_row[:, None, :], ang_col[:, None, :])
            kr = rope2d(kr, ang_row, ang_col)
        return qn, qr, kn, kr, v

    qn, qr, kn, kr, v = project(p_lat, True)
    qn_c, qr_c, kn_c, kr_c, v_c = project(p_ctx, False)
    B, S = qn.shape[:2]
    nb = S // Q_BLOCK

    def blocks(a):
        return a.reshape(B, nb, Q_BLOCK, *a.shape[2:]).swapaxes(0, 1)

    def attend_block(args):
        qn_i, qr_i = args
        s_lat = jnp.einsum('bqhd,bkhd->bhqk', qn_i, kn) + jnp.einsum('bqhr,bkr->bhqk', qr_i, kr)
        s_ctx = jnp.einsum('bqhd,bkhd->bhqk', qn_i, kn_c) + jnp.einsum('bqhr,bkr->bhqk', qr_i, kr_c)
        s = jnp.concatenate([s_lat, s_ctx], axis=-1).astype(jnp.float32) * scale
        p = jax.nn.softmax(s, axis=-1).astype(v.dtype)
        return (jnp.einsum('bhqk,bkhv->bqhv', p[..., :S], v)
                + jnp.einsum('bhqk,bkhv->bqhv', p[..., S:], v_c))

    o = lax.map(attend_block, (blocks(qn), blocks(qr)))
    lat = o.swapaxes(0, 1).reshape(B, S, MLA_WIDTH)
    ctx_out = None
    if need_ctx_out:
        L = qn_c.shape[1]
        s = (jnp.einsum('bqhd,bkhd->bhqk', qn_c, kn_c)
             + jnp.einsum('bqhr,bkr->bhqk', qr_c, kr_c)).astype(jnp.float32) * scale
        p = jax.nn.softmax(s, axis=-1).astype(v_c.dtype)
        ctx_out = jnp.einsum('bhqk,bkhv->bqhv', p, v_c).reshape(B, L, MLA_WIDTH)
    return lat, ctx_out


def hybrid_mixer(u_lat, u_ctx, ang_row, ang_col, w_in, w_gk_f, b_gk_f, w_gk_b, b_gk_b, gla_norm_g,
                 mla_q_norm_g, w_uq, mla_kv_norm_g, w_ukv, w_o, need_ctx_out):
    offsets = np.cumsum(IN_SPLITS)[:-1].tolist()
    p_lat = jnp.split(u_lat @ w_in, offsets, axis=-1)
    p_ctx = jnp.split(u_ctx @ w_in, offsets, axis=-1)
    g_lat, g_ctx = gla_group(p_lat[:6], p_ctx[:6], w_gk_f, b_gk_f, w_gk_b, b_gk_b, gla_norm_g, need_ctx_out)
    m_lat, m_ctx = mla_group(p_lat[6:], p_ctx[6:], ang_row, ang_col, mla_q_norm_g, w_uq,
                             mla_kv_norm_g, w_ukv, need_ctx_out)
    y_lat = jnp.concatenate([g_lat, m_lat], axis=-1) @ w_o
    y_ctx = jnp.concatenate([g_ctx, m_ctx], axis=-1) @ w_o if need_ctx_out else None
    return y_lat, y_ctx


def hier_moe(h, w_rg, b_rg, w_re, b_re, w_gate, w_up, w_down):
    shape = h.shape
    hf = h.reshape(-1, shape[-1])
    n = hf.shape[0]
    g_prob = jax.nn.softmax((hf @ w_rg + b_rg).astype(jnp.float32), axis=-1)
    p_g, g_top = lax.top_k(g_prob, 1)
    e_logits = (hf @ w_re + b_re).astype(jnp.float32).reshape(n, N_GROUPS, EXPERTS_PER_GROUP)
    e_in = e_logits[jnp.arange(n), g_top[:, 0]]
    top_p, top_i = lax.top_k(jax.nn.softmax(e_in, axis=-1), TOP_K)
    top_w = top_p / top_p.sum(-1, keepdims=True) * p_g
    expert_id = g_top * EXPERTS_PER_GROUP + top_i
    combine = jnp.einsum('nk,nke->ne', top_w,
                         jax.nn.one_hot(expert_id, N_EXPERTS, dtype=jnp.float32)).astype(h.dtype)
    out = jnp.zeros_like(hf)
    for e in range(N_EXPERTS):
        he = jax.nn.silu(hf @ w_gate[e]) * (hf @ w_up[e])
        out = out + combine[:, e:e + 1] * (he @ w_down[e])
    return out.reshape(shape)


def setup_inputs(seed: int = 0) -> dict:
    key = jax.random.key(seed)
    ks = jax.random.split(key, 32)
    f32 = jnp.float32

    def nrm(k, shape, s):
        return jax.random.normal(k, shape, f32) * s

    def gain(k, shape):
        return 1.0 + 0.02 * jax.random.normal(k, shape, f32)

    D = D_MODEL
    return {
        "x": nrm(ks[0], (BATCH, SEQ, D), 1.0),
        "c": nrm(ks[1], (BATCH, D), 1.0),
        "ctx": nrm(ks[2], (BATCH, CTX_LEN, D), 1.0),
        "c_ctx": nrm(ks[3], (D,), 1.0),
        "w_ada": nrm(ks[4], (DEPTH, D, 6 * D), 0.5 * D ** -0.5),
        "b_ada": nrm(ks[5], (DEPTH, 6 * D), 0.02),
        "w_in": nrm(ks[6], (DEPTH, D, IN_WIDTH), D ** -0.5),
        "w_gk_f": nrm(ks[7], (DEPTH, GLA_GATE_RANK, GLA_HEADS * GLA_DK), GLA_GATE_RANK ** -0.5),
        "b_gk_f": nrm(ks[8], (DEPTH, GLA_HEADS * GLA_DK), 0.1),
        "w_gk_b": nrm(ks[9], (DEPTH, GLA_GATE_RANK, GLA_HEADS * GLA_DK), GLA_GATE_RANK ** -0.5),
        "b_gk_b": nrm(ks[10], (DEPTH, GLA_HEADS * GLA_DK), 0.1),
        "gla_norm_g": gain(ks[11], (DEPTH, GLA_DV)),
        "mla_q_norm_g": gain(ks[12], (DEPTH, MLA_Q_RANK)),
        "w_uq": nrm(ks[13], (DEPTH, MLA_Q_RANK, MLA_HEADS * (MLA_NOPE + MLA_ROPE)), MLA_Q_RANK ** -0.5),
        "mla_kv_norm_g": gain(ks[14], (DEPTH, MLA_KV_RANK)),
        "w_ukv": nrm(ks[15], (DEPTH, MLA_KV_RANK, MLA_HEADS * (MLA_NOPE + MLA_DV)), MLA_KV_RANK ** -0.5),
        "w_o": nrm(ks[16], (DEPTH, MIX_WIDTH, D), DEEPNORM_BETA * MIX_WIDTH ** -0.5),
        "ln1_g": gain(ks[17], (DEPTH, D)),
        "ln1_b": nrm(ks[18], (DEPTH, D), 0.02),
        "w_router_group": nrm(ks[19], (DEPTH, D, N_GROUPS), D ** -0.5),
        "b_router_group": nrm(ks[20], (DEPTH, N_GROUPS), 0.01),
        "w_router_expert": nrm(ks[21], (DEPTH, D, N_EXPERTS), D ** -0.5),
        "b_router_expert": nrm(ks[22], (DEPTH, N_EXPERTS), 0.01),
        "w_expert_gate": nrm(ks[23], (DEPTH, N_EXPERTS, D, D_EXPERT), D ** -0.5),
        "w_expert_up": nrm(ks[24], (DEPTH, N_EXPERTS, D, D_EXPERT), D ** -0.5),
        "w_expert_down": nrm(ks[25], (DEPTH, N_EXPERTS, D_EXPERT, D), DEEPNORM_BETA * D_EXPERT ** -0.5),
        "ln2_g": gain(ks[26], (DEPTH, D)),
        "ln2_b": nrm(ks[27], (DEPTH, D), 0.02),
    }


def reference(x, c, ctx, c_ctx, w_ada, b_ada, w_in, w_gk_f, b_gk_f, w_gk_b, b_gk_b, gla_norm_g,
              mla_q_norm_g, w_uq, mla_kv_norm_g, w_ukv, w_o, ln1_g, ln1_b, w_router_group,
              b_router_group, w_router_expert, b_router_expert, w_expert_gate, w_expert_up,
              w_expert_down, ln2_g, ln2_b):
    seq_len = x.shape[1]
    rows = seq_len // GRID_W
    row, col = jnp.meshgrid(jnp.arange(rows), jnp.arange(GRID_W), indexing='ij')
    inv_freq = ROPE_BASE ** (-jnp.arange(ROPE_AXIS_PAIRS, dtype=jnp.float32) / ROPE_AXIS_PAIRS)
    ang_row = row.reshape(-1)[:, None].astype(jnp.float32) * inv_freq
    ang_col = col.reshape(-1)[:, None].astype(jnp.float32) * inv_freq

    for l in range(DEPTH):
        last = l == DEPTH - 1
        m_lat = jnp.split((jax.nn.silu(c) @ w_ada[l] + b_ada[l])[:, None, :], 6, axis=-1)
        m_ctx = jnp.split(jax.nn.silu(c_ctx) @ w_ada[l] + b_ada[l], 6, axis=-1)

        u_lat = modulate(x, m_lat[0], m_lat[1])
        u_ctx = modulate(ctx, m_ctx[0], m_ctx[1])
        y_lat, y_ctx = hybrid_mixer(u_lat, u_ctx, ang_row, ang_col, w_in[l], w_gk_f[l], b_gk_f[l],
                                    w_gk_b[l], b_gk_b[l], gla_norm_g[l], mla_q_norm_g[l], w_uq[l],
                                    mla_kv_norm_g[l], w_ukv[l], w_o[l], not last)
        x = layernorm(DEEPNORM_ALPHA * x + m_lat[2] * y_lat, ln1_g[l], ln1_b[l])

        h = modulate(x, m_lat[3], m_lat[4])
        y = hier_moe(h, w_router_group[l], b_router_group[l], w_router_expert[l], b_router_expert[l],
                     w_expert_gate[l], w_expert_up[l], w_expert_down[l])
        x = layernorm(DEEPNORM_ALPHA * x + m_lat[5] * y, ln2_g[l], ln2_b[l])

        if not last:
            ctx = layernorm(DEEPNORM_ALPHA * ctx + m_ctx[2] * y_ctx, ln1_g[l], ln1_b[l])
            hc = modulate(ctx, m_ctx[3], m_ctx[4])
            yc = hier_moe(hc, w_router_group[l], b_router_group[l], w_router_expert[l],
                          b_router_expert[l], w_expert_gate[l], w_expert_up[l], w_expert_down[l])
            ctx = layernorm(DEEPNORM_ALPHA * ctx + m_ctx[5] * yc, ln2_g[l], ln2_b[l])
    return x
```

```python
import math
from contextlib import ExitStack

import numpy as np
import concourse.bass as bass
import concourse.mybir as mybir
from concourse.bass_utils import run_bass_kernel_spmd

F32 = mybir.dt.float32
BF16 = mybir.dt.bfloat16
I32 = mybir.dt.int32
ALU = mybir.AluOpType
AF = mybir.ActivationFunctionType
AX = mybir.AxisListType

ENGS = ("pe", "act", "dve", "pool", "sp")

D = 1024
S = 4096
CT = 256
T = S + CT
NT = T // 128
EPS = 1e-6
ALPHA = 2.0 ** 0.25
NE = 16
DEV_NBLK = 99
DEV_STAGE = 99
DEV_G = 3
DEV_HQ = "pool"
DEV_NHE = 2
DEV_HEDX = True
DEV_DBGC = False
DEV_B = 4
DEV_BE = 16
DEV_A4 = 32
DEV_AH = 8
DEV_AQ = 8
DEV_GA = 2
DEV_GN = 99
DEV_RQ = True


class Res:
    __slots__ = ("name", "w", "ws", "rs")

    def __init__(self, name=""):
        self.name = name
        self.w = None
        self.ws = []
        self.rs = []


class Op:
    __slots__ = ("eng", "fn", "deps", "dma", "sig", "sem", "val", "prev_dma")

    def __init__(self, eng, fn, dma):
        self.eng = eng
        self.fn = fn
        self.deps = []
        self.dma = dma
        self.sig = dma
        self.sem = None
        self.val = 0
        self.prev_dma = None


class Prog:
    def __init__(self, nc, n_dma_sems=28):
        self.nc = nc
        self.ops = {e: [] for e in ENGS}
        self.all = []
        self.n_dma_sems = n_dma_sems
        self.pending = {e: [] for e in ENGS}
        self.dma_since_barrier = []
        self.default_q = "sp"

    def add(self, eng, fn, reads=(), writes=(), pwrites=(), dma=False):
        op = Op(eng, fn, dma)
        deps = []
        seen = set()

        def dep(d):
            if d is not None and id(d) not in seen:
                seen.add(id(d))
                deps.append(d)

        for r in reads:
            dep(r.w)
            for w in r.ws:
                dep(w)
        for w in writes:
            dep(w.w)
            for x in w.ws:
                dep(x)
            for rd in w.rs:
                dep(rd)
        for w in pwrites:
            dep(w.w)
            for rd in w.rs:
                dep(rd)
        for d in self.pending[eng]:
            dep(d)
        self.pending[eng] = []
        for d in deps:
            d.sig = True
            op.deps.append(d)
        for r in reads:
            r.rs.append(op)
        for w in writes:
            w.w = op
            w.ws = []
            w.rs = []
        for w in pwrites:
            w.ws.append(op)
        self.ops[eng].append(op)
        self.all.append(op)
        if dma:
            self.dma_since_barrier.append(op)
        return op

    def pe(self, fn, reads=(), writes=(), pwrites=()):
        return self.add("pe", fn, reads, writes, pwrites)

    def act(self, fn, reads=(), writes=(), pwrites=()):
        return self.add("act", fn, reads, writes, pwrites)

    def dve(self, fn, reads=(), writes=(), pwrites=()):
        return self.add("dve", fn, reads, writes, pwrites)

    def pool(self, fn, reads=(), writes=(), pwrites=()):
        return self.add("pool", fn, reads, writes, pwrites)

    def dma(self, fn, reads=(), writes=(), pwrites=(), q=None):
        return self.add(q or self.default_q, fn, reads, writes, pwrites, dma=True)

    def barrier(self):
        deps = list(self.dma_since_barrier)
        for e in ENGS:
            for op in reversed(self.ops[e]):
                if not op.dma:
                    deps.append(op)
                    break
        self.dma_since_barrier = []
        for e in ENGS:
            self.pending[e] = list(self.pending[e]) + deps

    def emit(self, final_ops=()):
        nc = self.nc
        with ExitStack() as st:
            esem = {e: st.enter_context(nc.semaphore("s_" + e)) for e in ENGS}
            dsems = {
                q: [st.enter_context(nc.semaphore("d_%s_%d" % (q, i))) for i in range(self.n_dma_sems)]
                for q in ("sp", "act", "pool")
            }
            cnt = {e: 0 for e in ENGS}
            dcnt = {q: 0 for q in dsems}
            duse = {q: [0] * self.n_dma_sems for q in dsems}
            dlast = {q: [None] * self.n_dma_sems for q in dsems}
            for op in self.all:
                if op.dma:
                    q = op.eng
                    k = dcnt[q] % self.n_dma_sems
                    dcnt[q] += 1
                    duse[q][k] += 1
                    op.sem = dsems[q][k]
                    op.val = 16 * duse[q][k]
                    op.prev_dma = dlast[q][k]
                    dlast[q][k] = op
                elif op.sig:
                    cnt[op.eng] += 1
                    op.sem = esem[op.eng]
                    op.val = cnt[op.eng]
            self.stats = dict(cnt=cnt, dcnt=dcnt, nops={e: len(v) for e, v in self.ops.items()})
            finals = list(final_ops)
            with nc.Block() as block:

                def stream(eng_name):
                    def body(eng):
                        seen = {}
                        nwait = 0
                        for op in self.ops[eng_name]:
                            dl = list(op.deps)
                            if op.dma and op.prev_dma is not None:
                                dl.append(op.prev_dma)
                            for d in dl:
                                key = id(d.sem)
                                if seen.get(key, 0) >= d.val:
                                    continue
                                seen[key] = d.val
                                eng.wait_ge(d.sem, d.val)
                                nwait += 1
                            ins = op.fn(eng)
                            if op.dma:
                                ins.then_inc(op.sem, 16)
                            elif op.sig:
                                ins.then_inc(op.sem, 1)
                        if eng_name == "sp":
                            for d in finals:
                                key = id(d.sem)
                                if seen.get(key, 0) >= d.val:
                                    continue
                                seen[key] = d.val
                                eng.wait_ge(d.sem, d.val)
                        self.stats["wait_" + eng_name] = nwait

                    return body

                block.tensor(stream("pe"))
                block.scalar(stream("act"))
                block.vector(stream("dve"))
                block.gpsimd(stream("pool"))
                block.sync(stream("sp"))


class Rot:
    def __init__(self, items):
        self.items = items
        self.i = 0

    def next(self):
        it = self.items[self.i % len(self.items)]
        self.i += 1
        return it


def sap(h, off, dims):
    return bass.AP(h, off, [list(d) for d in dims])


def build(debug=False, upto=99):
    nc = bass.Bass("TRN2", target_bir_lowering=False)
    P = Prog(nc)
    dbg_kind = "ExternalOutput" if debug else "Internal"

    def din(name, shape):
        return nc.dram_tensor(name, list(shape), F32, kind="ExternalInput")

    x_h = din("x", [S, D])
    ctx_h = din("ctx", [CT, D])
    c_h = din("c", [D])
    cctx_h = din("c_ctx", [D])
    wada_h = din("w_ada", [D, 6 * D])
    bada_h = din("b_ada", [6 * D])
    win_h = din("w_in", [D, 1984])
    wgk_h = din("wgk", [33, 512])
    glag_h = din("gla_g", [128])
    qg_h = din("q_g", [256])
    wuq_h = din("w_uq", [256, 768])
    kvg_h = din("kv_g", [128])
    wukv_h = din("w_ukv", [128, 1024])
    wo_h = din("w_o", [D, D])
    ln1g_h = din("ln1_g", [D])
    ln1b_h = din("ln1_b", [D])
    ln2g_h = din("ln2_g", [D])
    ln2b_h = din("ln2_b", [D])
    wr_h = din("w_r", [D, 20])
    br_h = din("b_r", [20])
    weg_h = din("w_eg", [NE, D, 256])
    weu_h = din("w_eu", [NE, D, 256])
    wed_h = din("w_ed", [NE, 256, D])
    out_h = nc.dram_tensor("out", [S, D], F32, kind="ExternalOutput")

    def scratch(name, shape, dt):
        return nc.dram_tensor(name, list(shape), dt, kind=dbg_kind)

    GQT = scratch("GQT", [256, T], BF16)
    GKT = scratch("GKT", [256, T], BF16)
    GKD = scratch("GKD", [T, 256], BF16)
    GVD = scratch("GVD", [T, 512], BF16)
    GRD = scratch("GRD", [S, 512], BF16)
    SPD = scratch("SPD", [T, 512], BF16)
    QTD = scratch("QTD", [8, 96, S], BF16)
    KNT = scratch("KNT", [4, 128, T], BF16)
    KRT = scratch("KRT", [32, T], BF16)
    VD = scratch("VD", [T, 512], BF16)
    OFD = scratch("OFD", [S, 512], F32)
    GLD = scratch("GLD", [S, 512], F32)
    MTD = scratch("MTD", [512, S], BF16)
    X1D = scratch("X1D", [S, D], F32)
    HED = scratch("HED", [NE, 256, S], BF16)
    R_ = {k: Res(k) for k in "GQT GKT GKD GVD GRD SPD QTD KNT KRT VD OFD GLD MTD X1D HED OUT".split()}

    with ExitStack() as gst:
        def gsb(name, shape, dt):
            return gst.enter_context(nc.sbuf_tensor(name, list(shape), dt))

        PSt = [gst.enter_context(nc.psum_tensor("ps%d" % i, [128, 1024], F32)) for i in range(4)]
        PSr = [[Res("ps%d_%d" % (i, j)) for j in range(2)] for i in range(4)]

        def bank(b):
            return PSt[b // 2], (b % 2) * 512, PSr[b // 2][b % 2]

        ident_f = gsb("ident_f", [128, 128], F32)
        ident_b = gsb("ident_b", [128, 128], BF16)
        ones_f = gsb("ones_f", [128, 128], F32)
        MODL = gsb("MODL", [128, 6 * D], F32)
        FM = gsb("FM", [128, 32], F32)
        r_ident = Res("ident")
        r_MODL = Res("MODL")
        r_FM = Res("FM")
        mk = {}
        diff = gsb("diff", [128, 128], F32)
        for _n in ("cum_f", "cum_b", "rem_f", "rem_b", "msk_f", "msk_b"):
            mk[_n] = gsb("mk_" + _n, [128, 128], BF16)

        with ExitStack() as st:
            def sb(name, shape, dt):
                return st.enter_context(nc.sbuf_tensor(name, list(shape), dt))

            io_i = sb("io_i", [128, 128], I32)
            r_diff = Res("diff")
            r_io = Res("io")
            P.pool(lambda e: e.iota(io_i[:], pattern=[[1, 128]], base=0, channel_multiplier=-1), writes=[r_io])
            P.dve(lambda e: e.tensor_copy(diff[:], io_i[:]), reads=[r_io], writes=[r_diff])
            P.dve(lambda e: e.tensor_single_scalar(ident_f[:], diff[:], 0.0, ALU.is_equal), reads=[r_diff], writes=[r_ident])
            P.dve(lambda e: e.tensor_copy(ident_b[:], ident_f[:]), reads=[r_ident], writes=[r_ident])
            P.dve(lambda e: e.memset(ones_f[:], 1.0), writes=[r_ident])
            for name, op, val in (("cum_f", ALU.is_ge, -1.0 / 16), ("cum_b", ALU.is_le, -1.0 / 16),
                                  ("rem_f", ALU.is_lt, -1.0 / 16), ("rem_b", ALU.is_gt, -1.0 / 16),
                                  ("msk_f", ALU.is_ge, 1.0), ("msk_b", ALU.is_le, 1.0)):
                m = mk[name]
                P.dve(lambda e, m=m, op=op, val=val: e.tensor_scalar(m[:], diff[:], 0.0, val, op, ALU.mult),
                      reads=[r_diff], writes=[r_ident])

            CTl = sb("CTl", [128, 2, 8], F32)
            SCf = sb("SCf", [128, 2, 8], F32)
            SC = sb("SC", [128, 2, 8, 128], F32)
            BADA = sb("BADA", [128, 6 * D], F32)
            MODC = sb("MODC", [128, 2 * D], F32)
            r_ct, r_sc, r_bada, r_modc = Res(), Res(), Res(), Res()
            for j, h in enumerate((c_h, cctx_h)):
                P.dma(lambda e, j=j, h=h: e.dma_start(out=CTl[:, j, :], in_=sap(h, 0, [[1, 128], [128, 8]]),
                                                     allow_slow_non_contiguous=True), pwrites=[r_ct])
            P.dma(lambda e: e.dma_start(out=BADA[:], in_=sap(bada_h, 0, [[0, 128], [1, 6 * D]])), writes=[r_bada])
            P.act(lambda e: e.activation(SCf[:], CTl[:], AF.Silu), reads=[r_ct], writes=[r_sc])
            P.dve(lambda e: e.tensor_copy(sap(SC, 0, [[2048, 128], [128, 16], [1, 128]]),
                                          sap(SCf, 0, [[16, 128], [1, 16], [0, 128]])), reads=[r_sc], writes=[r_sc])
            WA = Rot([(sb("WA%d" % i, [128, 8, 512], F32), Res()) for i in range(3)])
            wada_v = wada_h.ap().rearrange("(k p) n -> p k n", p=128)
            pb = Rot([bank(0), bank(1), bank(2), bank(3)])
            for g in range(12):
                wa, rwa = WA.next()
                P.dma(lambda e, wa=wa, g=g: e.dma_start(out=wa[:], in_=wada_v[:, :, g * 512:(g + 1) * 512]), writes=[rwa])
                for j in range(2):
                    if j == 1 and g >= 4:
                        continue
                    pt, po, pr = pb.next()
                    for k in range(8):
                        P.pe(lambda e, pt=pt, po=po, wa=wa, j=j, k=k: e.matmul(
                            pt[:, po:po + 512], SC[:, j, k, :], wa[:, k, :], start=(k == 0), stop=(k == 7)),
                            reads=[rwa, r_sc], writes=[pr] if k == 0 else (), pwrites=() if k == 0 else [pr])
                    dst = MODL if j == 0 else MODC
                    rd = r_MODL if j == 0 else r_modc
                    P.dve(lambda e, dst=dst, pt=pt, po=po, g=g: e.tensor_tensor(
                        dst[:, g * 512:(g + 1) * 512], pt[:, po:po + 512], BADA[:, g * 512:(g + 1) * 512], ALU.add),
                        reads=[pr, r_bada], pwrites=[rd])
            P.dve(lambda e: e.tensor_scalar_add(MODL[:, 1024:2048], MODL[:, 1024:2048], 1.0), reads=[r_MODL], writes=[r_MODL])
            P.dve(lambda e: e.tensor_scalar_add(MODL[:, 4096:5120], MODL[:, 4096:5120], 1.0), reads=[r_MODL], writes=[r_MODL])
            P.dve(lambda e: e.tensor_scalar_add(MODC[:, 1024:2048], MODC[:, 1024:2048], 1.0), reads=[r_modc], writes=[r_modc])
            for idx in range(32):
                src = MODL if idx < 16 else MODC
                rs = r_MODL if idx < 16 else r_modc
                col0 = (idx % 16) * 128
                pt, po, pr = pb.next()
                P.pe(lambda e, pt=pt, po=po, src=src, col0=col0: e.transpose(pt[:, po:po + 128], src[:, col0:col0 + 128], ident_f[:]),
                     reads=[rs, r_ident], writes=[pr])
                eng = P.dve if idx % 2 == 0 else P.act
                if idx % 2 == 0:
                    P.dve(lambda e, pt=pt, po=po, idx=idx: e.tensor_copy(FM[:, idx:idx + 1], pt[:, po:po + 1]), reads=[pr], pwrites=[r_FM])
                else:
                    P.act(lambda e, pt=pt, po=po, idx=idx: e.copy(FM[:, idx:idx + 1], pt[:, po:po + 1]), reads=[pr], pwrites=[r_FM])
            if debug:
                dbg_mod = nc.dram_tensor("dbg_mod", [128, 6 * D], F32, kind="ExternalOutput")
                dbg_fm = nc.dram_tensor("dbg_fm", [128, 32], F32, kind="ExternalOutput")
                P.dma(lambda e: e.dma_start(out=dbg_mod.ap(), in_=MODL[:]), reads=[r_MODL])
                P.dma(lambda e: e.dma_start(out=dbg_fm.ap(), in_=FM[:]), reads=[r_FM])
            P.barrier()

        if upto >= 1:
            with ExitStack() as st:
                def sb(name, shape, dt):
                    return st.enter_context(nc.sbuf_tensor(name, list(shape), dt))

                TB = sb("TB", [128, 2, S], BF16)
                r_tb = Res()
                with ExitStack() as st2:
                    pi = st2.enter_context(nc.sbuf_tensor("rp_pi", [96, 1], I32))
                    pf = st2.enter_context(nc.sbuf_tensor("rp_pf", [96, 4], F32))
                    ti = st2.enter_context(nc.sbuf_tensor("rp_ti", [96, 2, S], I32))
                    tf = st2.enter_context(nc.sbuf_tensor("rp_tf", [96, 2, S], F32))
                    tf2 = st2.enter_context(nc.sbuf_tensor("rp_tf2", [96, 2, S], F32))
                    pi2 = st2.enter_context(nc.sbuf_tensor("rp_pi2", [96, 1], I32))
                    r_rp = Res()
                    P.pool(lambda e: e.iota(pi[:], pattern=[[0, 1]], base=0, channel_multiplier=1), writes=[r_rp])
                    P.pool(lambda e: e.iota(ti[:, 0, :], pattern=[[1, 64], [0, 64]], base=0, channel_multiplier=0), pwrites=[r_rp])
                    P.pool(lambda e: e.iota(ti[:, 1, :], pattern=[[0, 64], [1, 64]], base=0, channel_multiplier=0), pwrites=[r_rp])
                    P.dve(lambda e: e.tensor_copy(pf[:, 0:1], pi[:]), reads=[r_rp], writes=[r_rp])
                    P.dve(lambda e: e.tensor_copy(tf[:], ti[:]), reads=[r_rp], writes=[r_rp])
                    P.dve(lambda e: e.tensor_single_scalar(pi2[:], pi[:], 7, ALU.bitwise_and), reads=[r_rp], writes=[r_rp])
                    P.dve(lambda e: e.tensor_copy(pf[:, 1:2], pi2[:]), reads=[r_rp], writes=[r_rp])
                    P.dve(lambda e: e.tensor_single_scalar(pi2[:], pi[:], 16, ALU.bitwise_and), reads=[r_rp], writes=[r_rp])
                    P.dve(lambda e: e.tensor_copy(pf[:, 2:3], pi2[:]), reads=[r_rp], writes=[r_rp])
                    P.dve(lambda e: e.tensor_scalar_mul(pf[:, 2:3], pf[:, 2:3], 1.0 / 16), reads=[r_rp], writes=[r_rp])
                    P.act(lambda e: e.activation(pf[:, 3:4], pf[:, 1:2], AF.Exp, scale=-math.log(10000.0) / 8.0), reads=[r_rp], writes=[r_rp])
                    P.dve(lambda e: e.tensor_tensor(tf[:, 1, :], tf[:, 1, :], tf[:, 0, :], ALU.subtract), reads=[r_rp], writes=[r_rp])
                    P.dve(lambda e: e.scalar_tensor_tensor(tf[:, 0, :], tf[:, 1, :], pf[:, 2:3], tf[:, 0, :], ALU.mult, ALU.add), reads=[r_rp], writes=[r_rp])
                    P.dve(lambda e: e.tensor_scalar_mul(tf[:, 0, :], tf[:, 0, :], pf[:, 3:4]), reads=[r_rp], writes=[r_rp])
                    P.dve(lambda e: e.tensor_scalar(tf[:, 1, :], tf[:, 0, :], 0.5 / math.pi, 0.25, ALU.mult, ALU.add), reads=[r_rp], writes=[r_rp])
                    P.dve(lambda e: e.tensor_scalar_mul(tf[:, 0, :], tf[:, 0, :], 0.5 / math.pi), reads=[r_rp], writes=[r_rp])
                    P.dve(lambda e: e.tensor_copy(ti[:], tf[:]), reads=[r_rp], writes=[r_rp])
                    P.dve(lambda e: e.tensor_copy(tf2[:], ti[:]), reads=[r_rp], writes=[r_rp])
                    P.dve(lambda e: e.tensor_tensor(tf[:], tf[:], tf2[:], ALU.subtract), reads=[r_rp], writes=[r_rp])
                    P.dve(lambda e: e.tensor_scalar_mul(tf[:], tf[:], 2 * math.pi), reads=[r_rp], writes=[r_rp])
                    P.dve(lambda e: e.tensor_scalar(tf2[:], tf[:], math.pi, -2 * math.pi, ALU.is_gt, ALU.mult), reads=[r_rp], writes=[r_rp])
                    P.dve(lambda e: e.tensor_tensor(tf[:], tf[:], tf2[:], ALU.add), reads=[r_rp], writes=[r_rp])
                    P.act(lambda e: e.activation(TB[0:96, 0, :], tf[:, 1, :], AF.Sin), reads=[r_rp], pwrites=[r_tb])
                    P.act(lambda e: e.activation(TB[0:96, 1, :], tf[:, 0, :], AF.Sin), reads=[r_rp], pwrites=[r_tb])
                    P.dve(lambda e: e.memset(TB[0:64, 0, :], 1.0), reads=[r_tb], writes=[r_tb])
                    P.dve(lambda e: e.memset(TB[0:64, 1, :], 0.0), reads=[r_tb], writes=[r_tb])
                    P.barrier()

                WIN = sb("WIN", [128, 8, 1984], BF16)
                WKRB = sb("WKRB", [128, 8, 96], BF16)
                WKRA = sb("WKRA", [128, 8, 96], BF16)
                WUQA = sb("WUQA", [128, 2, 768], BF16)
                WUQB = sb("WUQB", [128, 2, 768], BF16)
                QG = sb("QG", [128, 2], F32)
                KVG = sb("KVG", [128, 1], F32)
                WUKVK = sb("WUKVK", [128, 4, 128], BF16)
                WUKVV = sb("WUKVV", [128, 512], BF16)
                WGK = sb("WGK", [64, 512], BF16)
                XB = [sb("XB%d" % i, [128, 4, D], F32) for i in range(2)]
                UTs = [sb("UT%d" % i, [128, 8, 512], BF16) for i in range(2)]
                GTA = [sb("GTA%d" % i, [64, 512], BF16) for i in range(2)]
                CQT = sb("CQT", [128, 3, 512], BF16)
                SQ = sb("SQ", [128, 3, 512], F32)
                RQ = sb("RQ", [128, 512], F32)
                RKV = sb("RKV", [128, 512], F32)
                RKVT = sb("RKVT", [128, 4], F32)
                TBr = sb("TBr", [128, 2, 512], F32)
                STG = Rot([(sb("STG%d" % i, [128, 512], BF16), Res()) for i in range(2)])
                TMP = Rot([(sb("TMP%d" % i, [128, 512], F32), Res()) for i in range(4)])
                SPst = sb("SPst", [128, 4, 512], BF16)
                VDst = sb("VDst", [128, 4, 512], BF16)
                GKst = sb("GKst", [128, 4, 256], BF16)
                GVst = sb("GVst", [128, 4, 512], BF16)
                GRst = sb("GRst", [128, 4, 512], BF16)
                GQst = sb("GQst", [128, 2, 512], BF16)
                GK2st = sb("GK2st", [128, 2, 512], BF16)
                QTst = sb("QTst", [128, 8, 512], BF16)
                KNst = sb("KNst", [128, 4, 512], BF16)
                r_spst, r_vdst, r_gkst, r_gvst, r_grst, r_gqst, r_gk2st, r_qtst, r_knst = [Res() for _ in range(9)]
                r_w = Res()
                r_xb = [Res(), Res()]
                r_ut = [Res(), Res()]
                r_gta = [Res(), Res()]
                r_cqt, r_sq, r_rq, r_rkv, r_rkvt, r_tbr = Res(), Res(), Res(), Res(), Res(), Res()
                r_pt = [Res(), Res()]
                pb = Rot([bank(4), bank(5), bank(6), bank(7)])

                win_v = win_h.ap().rearrange("(k p) n -> p k n", p=128)
                WST = Rot([(XB[i], r_xb[i]) for i in range(2)])
                for k in range(8):
                    ws, rws = WST.next()
                    P.dma(lambda e, k=k, ws=ws: e.dma_start(out=sap(ws, 0, [[4096, 128], [1, 1984]]), in_=win_v[:, k, :]), writes=[rws])
                    if k % 2 == 0:
                        P.dve(lambda e, k=k, ws=ws: e.tensor_copy(WIN[:, k, :], sap(ws, 0, [[4096, 128], [1, 1984]])), reads=[rws], pwrites=[r_w])
                    else:
                        P.act(lambda e, k=k, ws=ws: e.copy(WIN[:, k, :], sap(ws, 0, [[4096, 128], [1, 1984]])), reads=[rws], pwrites=[r_w])
                ws, rws = WST.next()
                P.dma(lambda e, ws=ws: e.dma_start(out=sap(ws, 0, [[4096, 33], [1, 512]]), in_=wgk_h.ap()), writes=[rws])
                r_wgk = Res()
                P.dve(lambda e: e.memset(WGK[:], 0.0), writes=[r_wgk])
                P.dve(lambda e, ws=ws: e.tensor_copy(WGK[0:33, :], sap(ws, 0, [[4096, 33], [1, 512]])), reads=[rws, r_wgk], writes=[r_wgk])
                WUQf = sap(XB[0], 0, [[4096, 128], [768, 2], [1, 768]])
                WUKVf_h = XB[1]
                r_wq, r_wkv = r_xb[0], r_xb[1]
                P.dma(lambda e: e.dma_start(out=WUQf, in_=wuq_h.ap().rearrange("(k p) n -> p k n", p=128)), writes=[r_wq])
                P.dma(lambda e: e.dma_start(out=sap(WUKVf_h, 0, [[4096, 128], [1, 1024]]), in_=wukv_h.ap()), writes=[r_wkv])
                P.dma(lambda e: e.dma_start(out=QG[:], in_=sap(qg_h, 0, [[1, 128], [128, 2]]), allow_slow_non_contiguous=True), pwrites=[r_w])
                P.dma(lambda e: e.dma_start(out=KVG[:], in_=sap(kvg_h, 0, [[1, 128], [1, 1]])), pwrites=[r_w])
                r_w2 = Res()
                P.dve(lambda e: e.memset(WKRA[:], 0.0), writes=[r_w2])
                P.dve(lambda e: e.memset(WKRB[:], 0.0), reads=[r_w2], writes=[r_w2])
                P.dve(lambda e: e.tensor_copy(WKRA[:, :, 64:96], WIN[:, :, 1952:1984]), reads=[r_w, r_w2], writes=[r_w2])
                for a in range(2):
                    c0 = 1952 + a * 16
                    P.dve(lambda e, a=a, c0=c0: e.tensor_scalar_mul(WKRB[:, :, 64 + a * 16:64 + a * 16 + 8], WIN[:, :, c0 + 8:c0 + 16], -1.0), reads=[r_w, r_w2], writes=[r_w2])
                    P.dve(lambda e, a=a, c0=c0: e.tensor_copy(WKRB[:, :, 64 + a * 16 + 8:64 + a * 16 + 16], WIN[:, :, c0:c0 + 8]), reads=[r_w, r_w2], writes=[r_w2])
                P.dve(lambda e: e.memset(WUQB[:], 0.0), reads=[r_w2], writes=[r_w2])
                for kc in range(2):
                    P.dve(lambda e, kc=kc: e.tensor_scalar_mul(WUQA[:, kc, :], sap(XB[0], kc * 768, [[4096, 128], [1, 768]]), QG[:, kc:kc + 1]), reads=[r_w, r_wq], pwrites=[r_w2])
                for kc in range(2):
                    A3 = lambda off, kc=kc: sap(WUQA, kc * 768 + off, [[1536, 128], [96, 8], [1, 8]])
                    B3 = lambda off, kc=kc: sap(WUQB, kc * 768 + off, [[1536, 128], [96, 8], [1, 8]])
                    for a in range(2):
                        o = 64 + a * 16
                        P.dve(lambda e, o=o, A3=A3, B3=B3: e.tensor_scalar_mul(B3(o), A3(o + 8), -1.0), reads=[r_w2], pwrites=[r_w2])
                        P.dve(lambda e, o=o, A3=A3, B3=B3: e.tensor_copy(B3(o + 8), A3(o)), reads=[r_w2], pwrites=[r_w2])
                P.dve(lambda e: e.tensor_scalar_mul(sap(WUKVK, 0, [[512, 128], [64, 8], [1, 64]]),
                                                    sap(WUKVf_h, 0, [[4096, 128], [128, 8], [1, 64]]), KVG[:, 0:1]), reads=[r_w, r_wkv], pwrites=[r_w2])
                P.dve(lambda e: e.tensor_scalar_mul(sap(WUKVV, 0, [[512, 128], [64, 8], [1, 64]]),
                                                    sap(WUKVf_h, 64, [[4096, 128], [128, 8], [1, 64]]), KVG[:, 0:1]), reads=[r_w, r_wkv], pwrites=[r_w2])
                for i in range(2):
                    P.dve(lambda e, i=i: e.memset(GTA[i][:], 0.0), writes=[r_gta[i]])
                    P.dve(lambda e, i=i: e.memset(GTA[i][32:33, :], 1.0), writes=[r_gta[i]])

                blocks = [(0, 2, True)] + [(CT + 512 * b, 4, False) for b in range(8)]

                def src_rows(tok0, t):
                    if tok0 < CT:
                        return ctx_h.ap()[tok0 + t * 128: tok0 + (t + 1) * 128, :]
                    s0 = tok0 - CT + t * 128
                    return x_h.ap()[s0:s0 + 128, :]

                def load_block(bi):
                    tok0, ntl, _ = blocks[bi]
                    xb = XB[bi % 2]
                    if tok0 < CT:
                        src = ctx_h.ap().rearrange("(t p) d -> p t d", p=128)
                    else:
                        src = x_h.ap()[tok0 - CT:tok0 - CT + ntl * 128, :].rearrange("(t p) d -> p t d", p=128)
                    P.dma(lambda e, xb=xb, src=src, ntl=ntl: e.dma_start(out=xb[:, 0:ntl, :], in_=src), writes=[r_xb[bi % 2]])

                def mm_acc(pt, po, pr, M, N, lhs, rhs, nk, reads, f32=False):
                    for k in range(nk):
                        P.pe(lambda e, k=k: e.matmul(pt[0:M, po:po + N], lhs(k), rhs(k), start=(k == 0), stop=(k == nk - 1)),
                             reads=reads, writes=[pr] if k == 0 else (), pwrites=() if k == 0 else [pr])

                def do_block(bi, tok0, ntl, is_ctx):
                    if bi + 1 < len(blocks):
                        load_block(bi + 1)
                    NB = ntl * 128
                    s0 = tok0 - CT
                    xb, rxb = XB[bi % 2], r_xb[bi % 2]
                    UT, rut = UTs[bi % 2], r_ut[bi % 2]
                    gta, rgta = GTA[bi % 2], r_gta[bi % 2]
                    fsh, fsc = (16, 24) if is_ctx else (0, 8)
                    for t in range(ntl):
                        PT, rpt = PSt[t % 2], r_pt[t % 2]
                        for k in range(8):
                            P.pe(lambda e, PT=PT, xb=xb, t=t, k=k: e.transpose(PT[:, k * 128:(k + 1) * 128], xb[:, t, k * 128:(k + 1) * 128], ident_f[:]),
                                 reads=[rxb, r_ident], writes=[rpt] if k == 0 else (), pwrites=() if k == 0 else [rpt])
                        for k in range(8):
                            wr = dict(writes=[rut]) if (t == 0 and k == 0) else dict(pwrites=[rut])
                            if True:
                                P.dve(lambda e, PT=PT, UT=UT, t=t, k=k: e.tensor_scalar(
                                    UT[:, k, t * 128:(t + 1) * 128], PT[:, k * 128:(k + 1) * 128],
                                    FM[:, fsc + k:fsc + k + 1], FM[:, fsh + k:fsh + k + 1], ALU.mult, ALU.add), reads=[rpt, r_FM], **wr)
                            else:
                                P.act(lambda e, PT=PT, UT=UT, t=t, k=k: e.activation(
                                    UT[:, k, t * 128:(t + 1) * 128], PT[:, k * 128:(k + 1) * 128], AF.Identity,
                                    bias=FM[:, fsh + k:fsh + k + 1], scale=FM[:, fsc + k:fsc + k + 1]), reads=[rpt, r_FM], **wr)
                    if DEV_STAGE <= 1:
                        return
                    rw = [r_w, r_w2, rut]
                    U = lambda k: UT[:, k, 0:NB]
                    for c in range(4):
                        pt, po, pr = pb.next()
                        mm_acc(pt, po, pr, 128, NB, lambda k, c=c: WIN[:, k, c * 128:(c + 1) * 128], U, 8, rw)
                        cc = c % 2
                        wr = (lambda r: dict(writes=[r]) if cc == 0 else dict(pwrites=[r]))
                        if c < 2:
                            P.act(lambda e, pt=pt, po=po, cc=cc: e.activation(GQst[:, cc, 0:NB], pt[:, po:po + NB], AF.Copy, scale=0.125), reads=[pr], **wr(r_gqst))
                        else:
                            P.dve(lambda e, pt=pt, po=po, cc=cc: e.tensor_copy(GK2st[:, cc, 0:NB], pt[:, po:po + NB]), reads=[pr], **wr(r_gk2st))
                        if cc == 1:
                            stt, rst, dst, rdst = (GQst, r_gqst, GQT, R_["GQT"]) if c < 2 else (GK2st, r_gk2st, GKT, R_["GKT"])
                            P.dma(lambda e, stt=stt, dst=dst: e.dma_start(out=dst.ap().rearrange("(c p) t -> p c t", p=128)[:, :, tok0:tok0 + NB], in_=stt[:, :, 0:NB]),
                                  reads=[rst], pwrites=[rdst])
                    if DEV_STAGE <= 2:
                        return
                    pt, po, pr = pb.next()
                    mm_acc(pt, po, pr, 32, NB, lambda k: WIN[:, k, 1024:1056], U, 8, rw)
                    P.act(lambda e, pt=pt, po=po, gta=gta: e.copy(gta[0:32, 0:NB], pt[0:32, po:po + NB]), reads=[pr], writes=[rgta])
                    for t in range(ntl):
                        pt, po, pr = pb.next()
                        P.pe(lambda e, pt=pt, po=po, gta=gta, t=t: e.matmul(pt[:, po:po + 512], gta[0:64, t * 128:(t + 1) * 128], WGK[0:64, :], start=True, stop=True),
                             reads=[rgta, r_wgk], writes=[pr])
                        tm, rtm = TMP.next()
                        P.act(lambda e, tm=tm, pt=pt, po=po: e.activation(tm[:], pt[:, po:po + 512], AF.Exp, scale=-1.0), reads=[pr], writes=[rtm])
                        P.act(lambda e, tm=tm, t=t: e.activation(SPst[:, t, :], tm[:], AF.Ln, bias=1.0), reads=[rtm],
                              writes=[r_spst] if t == 0 else (), pwrites=() if t == 0 else [r_spst])
                    P.dma(lambda e: e.dma_start(out=SPD.ap()[tok0:tok0 + NB, :].rearrange("(t p) c -> p t c", p=128), in_=SPst[:, 0:ntl, :]),
                          reads=[r_spst], pwrites=[R_["SPD"]])
                    if DEV_STAGE <= 3:
                        return
                    for c in range(3):
                        if c < 2 and is_ctx:
                            continue
                        pt, po, pr = pb.next()
                        c0 = 1568 + c * 128
                        mm_acc(pt, po, pr, 128, NB, lambda k, c0=c0: WIN[:, k, c0:c0 + 128], U, 8, rw)
                        P.dve(lambda e, pt=pt, po=po, c=c: e.tensor_copy(CQT[:, c, 0:NB], pt[:, po:po + NB]), reads=[pr], pwrites=[r_cqt])
                        P.act(lambda e, pt=pt, po=po, c=c: e.activation(SQ[:, c, 0:NB], CQT[:, c, 0:NB], AF.Square), reads=[r_cqt], pwrites=[r_sq])
                    if (not is_ctx) and DEV_RQ:
                        pt, po, pr = pb.next()
                        mm_acc(pt, po, pr, 128, NB, lambda k: ones_f[:], lambda k: SQ[:, k, 0:NB], 2, [r_sq, r_ident])
                        P.dve(lambda e, pt=pt, po=po: e.tensor_scalar(RQ[:, 0:NB], pt[:, po:po + NB], 96.0 / 256, 96.0 * EPS, ALU.mult, ALU.add), reads=[pr], writes=[r_rq])
                        P.act(lambda e: e.activation(RQ[:, 0:NB], RQ[:, 0:NB], AF.Ln), reads=[r_rq], writes=[r_rq])
                        P.act(lambda e: e.activation(RQ[:, 0:NB], RQ[:, 0:NB], AF.Exp, scale=-0.5), reads=[r_rq], writes=[r_rq])
                    pt, po, pr = pb.next()
                    mm_acc(pt, po, pr, 128, NB, lambda k: ones_f[:], lambda k: SQ[:, 2, 0:NB], 1, [r_sq, r_ident])
                    P.dve(lambda e, pt=pt, po=po: e.tensor_scalar(RKV[:, 0:NB], pt[:, po:po + NB], 1.0 / 128, EPS, ALU.mult, ALU.add), reads=[pr], writes=[r_rkv])
                    P.act(lambda e: e.activation(RKV[:, 0:NB], RKV[:, 0:NB], AF.Ln), reads=[r_rkv], writes=[r_rkv])
                    P.act(lambda e: e.activation(RKV[:, 0:NB], RKV[:, 0:NB], AF.Exp, scale=-0.5), reads=[r_rkv], writes=[r_rkv])
                    pt, po, pr = pb.next()
                    for t in range(ntl):
                        P.pe(lambda e, pt=pt, po=po, t=t: e.matmul(pt[:, po + t * 128:po + (t + 1) * 128], SQ[:, 2, t * 128:(t + 1) * 128], ones_f[:], start=True, stop=True),
                             reads=[r_sq, r_ident], writes=[pr] if t == 0 else (), pwrites=() if t == 0 else [pr])
                    P.dve(lambda e, pt=pt, po=po: e.tensor_scalar(RKVT[:, 0:ntl], sap(pt, po, [[1024, 128], [128, ntl]]), 1.0 / 128, EPS, ALU.mult, ALU.add), reads=[pr], writes=[r_rkvt])
                    P.act(lambda e: e.activation(RKVT[:, 0:ntl], RKVT[:, 0:ntl], AF.Ln), reads=[r_rkvt], writes=[r_rkvt])
                    P.act(lambda e: e.activation(RKVT[:, 0:ntl], RKVT[:, 0:ntl], AF.Exp, scale=-0.5), reads=[r_rkvt], writes=[r_rkvt])
                    if DEV_STAGE <= 4:
                        return
                    if not is_ctx:
                        for j in range(2):
                            P.dve(lambda e, j=j: e.tensor_tensor(TBr[0:96, j, :], TB[0:96, j, s0:s0 + 512], RQ[0:96, :], ALU.mult),
                                  reads=[r_tb, r_rq], writes=[r_tbr] if j == 0 else (), pwrites=() if j == 0 else [r_tbr])
                        for h in range(8):
                            pa, poa, pra = pb.next()
                            mm_acc(pa, poa, pra, 96, 512, lambda k, h=h: WUQA[:, k, h * 96:(h + 1) * 96], lambda k: CQT[:, k, :], 2, [r_w2, r_cqt])
                            pb_, pob, prb = pb.next()
                            mm_acc(pb_, pob, prb, 96, 512, lambda k, h=h: WUQB[:, k, h * 96:(h + 1) * 96], lambda k: CQT[:, k, :], 2, [r_w2, r_cqt])
                            t1, rt1 = TMP.next()
                            t2, rt2 = TMP.next()
                            P.dve(lambda e, t1=t1, pa=pa, poa=poa: e.tensor_tensor(t1[0:96, :], pa[0:96, poa:poa + 512], TBr[0:96, 0, :], ALU.mult), reads=[pra, r_tbr], writes=[rt1])
                            P.dve(lambda e, t2=t2, pb_=pb_, pob=pob: e.tensor_tensor(t2[0:96, :], pb_[0:96, pob:pob + 512], TBr[0:96, 1, :], ALU.mult), reads=[prb, r_tbr], writes=[rt2])
                            P.dve(lambda e, h=h, t1=t1, t2=t2: e.tensor_tensor(QTst[0:96, h, :], t1[0:96, :], t2[0:96, :], ALU.add), reads=[rt1, rt2],
                                  writes=[r_qtst] if h == 0 else (), pwrites=() if h == 0 else [r_qtst])
                        P.dma(lambda e: e.dma_start(out=QTD.ap().rearrange("h r t -> r h t")[:, :, s0:s0 + 512], in_=QTst[0:96, :, :]), reads=[r_qtst], pwrites=[R_["QTD"]])
                    for pr_i in range(4):
                        pt, po, pr = pb.next()
                        mm_acc(pt, po, pr, 128, NB, lambda k, pr_i=pr_i: WUKVK[:, pr_i, :], lambda k: CQT[:, 2, 0:NB], 1, [r_w2, r_cqt])
                        P.dve(lambda e, pr_i=pr_i, pt=pt, po=po: e.tensor_tensor(KNst[:, pr_i, 0:NB], pt[:, po:po + NB], RKV[:, 0:NB], ALU.mult), reads=[pr, r_rkv],
                              writes=[r_knst] if pr_i == 0 else (), pwrites=() if pr_i == 0 else [r_knst])
                    P.dma(lambda e: e.dma_start(out=KNT.ap().rearrange("q r t -> r q t")[:, :, tok0:tok0 + NB], in_=KNst[:, :, 0:NB]), reads=[r_knst], pwrites=[R_["KNT"]])
                    if DEV_STAGE <= 5:
                        return
                    pa, poa, pra = pb.next()
                    mm_acc(pa, poa, pra, 96, NB, lambda k: WKRA[:, k, :], U, 8, rw)
                    sg, rsg = STG.next()
                    if is_ctx:
                        P.act(lambda e, sg=sg, pa=pa, poa=poa: e.copy(sg[0:96, 0:NB], pa[0:96, poa:poa + NB]), reads=[pra], writes=[rsg])
                    else:
                        pb_, pob, prb = pb.next()
                        mm_acc(pb_, pob, prb, 96, NB, lambda k: WKRB[:, k, :], U, 8, rw)
                        t1, rt1 = TMP.next()
                        t2, rt2 = TMP.next()
                        P.dve(lambda e, t1=t1, pa=pa, poa=poa: e.tensor_tensor(t1[0:96, :], pa[0:96, poa:poa + 512], TB[0:96, 0, s0:s0 + 512], ALU.mult), reads=[pra, r_tb], writes=[rt1])
                        P.dve(lambda e, t2=t2, pb_=pb_, pob=pob: e.tensor_tensor(t2[0:96, :], pb_[0:96, pob:pob + 512], TB[0:96, 1, s0:s0 + 512], ALU.mult), reads=[prb, r_tb], writes=[rt2])
                        P.dve(lambda e, sg=sg, t1=t1, t2=t2: e.tensor_tensor(sg[0:96, :], t1[0:96, :], t2[0:96, :], ALU.add), reads=[rt1, rt2], writes=[rsg])
                    P.dma(lambda e, sg=sg: e.dma_start(out=KRT.ap()[:, tok0:tok0 + NB], in_=sg[64:96, 0:NB]), reads=[rsg], pwrites=[R_["KRT"]])
                    if DEV_STAGE <= 6:
                        return
                    for t in range(ntl):
                        lhs = lambda k, t=t: UT[:, k, t * 128:(t + 1) * 128]
                        wrs = (lambda r: dict(writes=[r]) if t == 0 else dict(pwrites=[r]))
                        pt, po, pr = pb.next()
                        P.pe(lambda e, pt=pt, po=po, t=t: e.matmul(pt[:, po:po + 512], CQT[:, 2, t * 128:(t + 1) * 128], WUKVV[:], start=True, stop=True),
                             reads=[r_cqt, r_w2], writes=[pr])
                        P.dve(lambda e, pt=pt, po=po, t=t: e.tensor_scalar_mul(VDst[:, t, :], pt[:, po:po + 512], RKVT[:, t:t + 1]), reads=[pr, r_rkvt], **wrs(r_vdst))
                        pt, po, pr = pb.next()
                        mm_acc(pt, po, pr, 128, 256, lhs, lambda k: WIN[:, k, 256:512], 8, rw)
                        P.dve(lambda e, pt=pt, po=po, t=t: e.tensor_copy(GKst[:, t, :], pt[:, po:po + 256]), reads=[pr], **wrs(r_gkst))
                        pt, po, pr = pb.next()
                        mm_acc(pt, po, pr, 128, 512, lhs, lambda k: WIN[:, k, 512:1024], 8, rw)
                        P.act(lambda e, pt=pt, po=po, t=t: e.copy(GVst[:, t, :], pt[:, po:po + 512]), reads=[pr], **wrs(r_gvst))
                        if not is_ctx:
                            pt, po, pr = pb.next()
                            mm_acc(pt, po, pr, 128, 512, lhs, lambda k: WIN[:, k, 1056:1568], 8, rw)
                            P.act(lambda e, pt=pt, po=po, t=t: e.activation(GRst[:, t, :], pt[:, po:po + 512], AF.Silu), reads=[pr], **wrs(r_grst))
                    rows = lambda dram, r0: dram.ap()[r0:r0 + NB, :].rearrange("(t p) c -> p t c", p=128)
                    P.dma(lambda e: e.dma_start(out=rows(VD, tok0), in_=VDst[:, 0:ntl, :]), reads=[r_vdst], pwrites=[R_["VD"]])
                    P.dma(lambda e: e.dma_start(out=rows(GKD, tok0), in_=GKst[:, 0:ntl, :]), reads=[r_gkst], pwrites=[R_["GKD"]])
                    P.dma(lambda e: e.dma_start(out=rows(GVD, tok0), in_=GVst[:, 0:ntl, :]), reads=[r_gvst], pwrites=[R_["GVD"]])
                    if not is_ctx:
                        P.dma(lambda e: e.dma_start(out=rows(GRD, s0), in_=GRst[:, 0:ntl, :]), reads=[r_grst], pwrites=[R_["GRD"]])

                load_block(0)
                for bi, blk in enumerate(blocks[:DEV_NBLK]):
                    do_block(bi, *blk)
                P.barrier()

        def drive(gs):
            gs = list(gs)
            while gs:
                for g in list(gs):
                    try:
                        next(g)
                    except StopIteration:
                        gs.remove(g)

        if upto >= 2:
            P.default_q = "pool"
            OBD = scratch("OBD", [S, 512], F32)
            R_["OBD"] = Res("OBD")
            with ExitStack() as st:
                def sb(name, shape, dt):
                    return st.enter_context(nc.sbuf_tensor(name, list(shape), dt))

                QT2 = sb("QT2", [128, 2, T], BF16)
                KT2 = sb("KT2", [128, 2, T], BF16)
                KTM = sb("KTM", [128, NT, 256], BF16)
                VTM = sb("VTM", [128, NT, 512], BF16)
                SPT = sb("SPT", [128, NT, 512], BF16)
                r_in = Res()
                NBUF = 2
                GS_ = [sb("GS_%d" % d, [128, 256], F32) for d in range(2)]
                GSbl = [sb("GSbl%d" % d, [128, 256], BF16) for d in range(2)]
                GSbh = [sb("GSbh%d" % d, [128, 256], BF16) for d in range(2)]
                r_S = [Res(), Res()]
                r_Sb = [Res(), Res()]
                mkb = lambda nm, shp, dt: [[sb("%s%d_%d" % (nm, d, i), shp, dt) for i in range(NBUF)] for d in range(2)]
                gEQ = mkb("EQ", [128, 256], F32); gEK = mkb("EK", [128, 256], F32); gEKD = mkb("EKD", [128, 256], F32)
                gQE = mkb("QE", [128, 256], BF16); gKEl = mkb("KEl", [128, 256], BF16); gKEh = mkb("KEh", [128, 256], BF16)
                gKD = mkb("KD", [128, 256], BF16); gAT = mkb("AT", [128, 512], BF16)
                gr_e = [[Res() for _ in range(NBUF)] for _ in range(2)]
                gr_qk = [[Res() for _ in range(NBUF)] for _ in range(2)]
                gr_at = [[Res() for _ in range(NBUF)] for _ in range(2)]
                gOST = [sb("OST%d" % d, [128, 4, 512], F32) for d in range(2)]
                gr_ost = [Res(), Res()]

                for p2 in range(2):
                    P.dma(lambda e, p2=p2: e.dma_start(out=QT2[:, p2, :], in_=GQT.ap()[p2 * 128:(p2 + 1) * 128, :]), reads=[R_["GQT"]], pwrites=[r_in])
                    P.dma(lambda e, p2=p2: e.dma_start(out=KT2[:, p2, :], in_=GKT.ap()[p2 * 128:(p2 + 1) * 128, :]), reads=[R_["GKT"]], pwrites=[r_in])
                P.dma(lambda e: e.dma_start(out=KTM[:], in_=GKD.ap().rearrange("(n p) c -> p n c", p=128)), reads=[R_["GKD"]], pwrites=[r_in])
                P.dma(lambda e: e.dma_start(out=VTM[:], in_=GVD.ap().rearrange("(n p) c -> p n c", p=128)), reads=[R_["GVD"]], pwrites=[r_in])
                P.dma(lambda e: e.dma_start(out=SPT[:], in_=SPD.ap().rearrange("(n p) c -> p n c", p=128)), reads=[R_["SPD"]], pwrites=[r_in])
                for d in range(2):
                    for i in range(NBUF):
                        P.dve(lambda e, d=d, i=i: e.memset(gKEl[d][i][:], 0.0), writes=[gr_qk[d][i]])
                        P.dve(lambda e, d=d, i=i: e.memset(gKEh[d][i][:], 0.0), pwrites=[gr_qk[d][i]])

                def gla_dir(d):
                    cum, rem, msk = (mk["cum_f"], mk["rem_f"], mk["msk_f"]) if d == 0 else (mk["cum_b"], mk["rem_b"], mk["msk_b"])
                    lastcol = 127 if d == 0 else 0
                    order = list(range(NT)) if d == 0 else [1, 0] + list(range(NT - 1, 1, -1))
                    S_, Sbl, Sbh = GS_[d], GSbl[d], GSbh[d]
                    OD, rOD = (OFD, R_["OFD"]) if d == 0 else (OBD, R_["OBD"])
                    ost, rost = gOST[d], gr_ost[d]
                    bk = lambda j: bank(4 * d + j)
                    P.dve(lambda e: e.memset(S_[:], 0.0), writes=[r_S[d]])
                    P.dve(lambda e: e.memset(Sbl[:], 0.0), writes=[r_Sb[d]])
                    P.dve(lambda e: e.memset(Sbh[:], 0.0), pwrites=[r_Sb[d]])
                    yield

                    def pre(i):
                        tt = order[i]
                        b = i % NBUF
                        lat = tt >= 2
                        t0 = tt * 128
                        EQ, EK, EKD, QE, KEl, KEh, KD, AT = gEQ[d][b], gEK[d][b], gEKD[d][b], gQE[d][b], gKEl[d][b], gKEh[d][b], gKD[d][b], gAT[d][b]
                        r_e, r_qk, r_at = gr_e[d][b], gr_qk[d][b], gr_at[d][b]
                        pbt, pbo, pbr = bk(0)
                        for p in range(2):
                            P.pe(lambda e, p=p: e.matmul(pbt[:, pbo + p * 128:pbo + (p + 1) * 128], SPT[:, tt, d * 256 + p * 128:d * 256 + (p + 1) * 128], cum[:], start=True, stop=True),
                                 reads=[r_in, r_ident], writes=[pbr] if p == 0 else (), pwrites=() if p == 0 else [pbr])
                        P.pe(lambda e: e.matmul(pbt[:, pbo + 256:pbo + 512], rem[:], SPT[:, tt, d * 256:(d + 1) * 256], start=True, stop=True),
                             reads=[r_in, r_ident], pwrites=[pbr])
                        yield
                        P.act(lambda e: e.activation(EQ[:], pbt[:, pbo:pbo + 256], AF.Exp), reads=[pbr], writes=[r_e])
                        P.act(lambda e: e.activation(EK[:], pbt[:, pbo:pbo + 256], AF.Exp, scale=-1.0), reads=[pbr], pwrites=[r_e])
                        P.act(lambda e: e.activation(EKD[:], pbt[:, pbo + 256:pbo + 512], AF.Exp), reads=[pbr], pwrites=[r_e])
                        yield
                        v3 = lambda h_: sap(h_, 0, [[256, 128], [128, 2], [1, 128]])
                        v3h = lambda h_, r0: sap(h_, r0 * 256, [[256, 64], [128, 2], [1, 128]])
                        P.dve(lambda e: e.tensor_tensor(v3(QE), QT2[:, :, t0:t0 + 128], v3(EQ), ALU.mult), reads=[r_in, r_e], writes=[r_qk])
                        P.dve(lambda e: e.tensor_tensor(v3h(KEl, 0), KT2[0:64, :, t0:t0 + 128], v3h(EK, 0), ALU.mult), reads=[r_in, r_e], pwrites=[r_qk])
                        yield
                        P.dve(lambda e: e.tensor_tensor(v3h(KEh, 64), KT2[64:128, :, t0:t0 + 128], v3h(EK, 64), ALU.mult), reads=[r_in, r_e], pwrites=[r_qk])
                        P.dve(lambda e: e.tensor_tensor(KD[:], KTM[:, tt, :], EKD[:], ALU.mult), reads=[r_in, r_e], pwrites=[r_qk])
                        yield
                        if lat:
                            pat, pao, par = bk(1)
                            for h in range(4):
                                r0, c0 = (h % 2) * 64, (h // 2) * 128
                                P.pe(lambda e, h=h, r0=r0, c0=c0: e.matmul(pat[:, pao + h * 128:pao + (h + 1) * 128], (KEl if r0 == 0 else KEh)[:, c0:c0 + 128], QE[:, c0:c0 + 128], start=True, stop=True),
                                     reads=[r_qk], writes=[par] if h == 0 else (), pwrites=() if h == 0 else [par])
                            yield
                            P.dve(lambda e: e.tensor_tensor(sap(AT, 0, [[512, 128], [128, 4], [1, 128]]), sap(pat, pao, [[1024, 128], [128, 4], [1, 128]]),
                                                            sap(msk, 0, [[128, 128], [0, 4], [1, 128]]), ALU.mult), reads=[par, r_ident], writes=[r_at])
                            yield

                    def post(i):
                        tt = order[i]
                        b = i % NBUF
                        lat = tt >= 2
                        EQ, QE, KD, AT = gEQ[d][b], gQE[d][b], gKD[d][b], gAT[d][b]
                        r_e, r_qk, r_at = gr_e[d][b], gr_qk[d][b], gr_at[d][b]
                        pot, poo, por = bk(2)
                        pdt, pdo, pdr = bk(3)
                        if lat:
                            for h in range(4):
                                r0, c0 = (h % 2) * 64, (h // 2) * 128
                                P.pe(lambda e, h=h: e.matmul(pot[:, poo + h * 128:poo + (h + 1) * 128], AT[:, h * 128:(h + 1) * 128], VTM[:, tt, h * 128:(h + 1) * 128], start=True, stop=False),
                                     reads=[r_at, r_in], writes=[por] if h == 0 else (), pwrites=() if h == 0 else [por])
                                P.pe(lambda e, h=h, r0=r0, c0=c0: e.matmul(pot[:, poo + h * 128:poo + (h + 1) * 128], QE[:, c0:c0 + 128], (Sbl if r0 == 0 else Sbh)[:, c0:c0 + 128], start=False, stop=True),
                                     reads=[r_qk, r_Sb[d]], pwrites=[por])
                        for h in range(4):
                            r0, c0 = (h % 2) * 64, (h // 2) * 128
                            P.pe(lambda e, h=h, r0=r0, c0=c0: e.matmul(pdt[r0:r0 + 64, pdo + c0:pdo + c0 + 128], KD[:, h * 64:(h + 1) * 64], VTM[:, tt, h * 128:(h + 1) * 128], start=True, stop=True),
                                 reads=[r_qk, r_in], writes=[pdr] if h == 0 else (), pwrites=() if h == 0 else [pdr])
                        yield
                        for p in range(2):
                            col = p * 128 + lastcol
                            P.dve(lambda e, p=p, col=col: e.scalar_tensor_tensor(S_[:, p * 128:(p + 1) * 128], S_[:, p * 128:(p + 1) * 128], EQ[:, col:col + 1],
                                                                                 pdt[:, pdo + p * 128:pdo + (p + 1) * 128], ALU.mult, ALU.add),
                                  reads=[pdr, r_e, r_S[d]], writes=[r_S[d]])
                        yield
                        P.act(lambda e: e.copy(Sbl[0:64, :], S_[0:64, :]), reads=[r_S[d]], writes=[r_Sb[d]])
                        P.act(lambda e: e.copy(Sbh[64:128, :], S_[64:128, :]), reads=[r_S[d]], pwrites=[r_Sb[d]])
                        if lat:
                            lt = tt - 2
                            slot = lt % 4
                            first = (slot == 0) if d == 0 else (slot == 3)
                            last = (slot == 3) if d == 0 else (slot == 0)
                            P.act(lambda e: e.copy(ost[:, slot, :], pot[:, poo:poo + 512]), reads=[por], writes=[rost] if first else (), pwrites=() if first else [rost])
                            if last:
                                g0 = (lt // 4) * 512
                                P.dma(lambda e: e.dma_start(out=OD.ap()[g0:g0 + 512, :].rearrange("(t p) c -> p t c", p=128), in_=ost[:]), reads=[rost], pwrites=[rOD])
                        yield

                    yield from pre(0)
                    for i in range(NT):
                        gens = []
                        if i + 1 < NT:
                            gens.append(pre(i + 1))
                        gens.append(post(i))
                        while gens:
                            for g in list(gens):
                                try:
                                    next(g)
                                    yield
                                except StopIteration:
                                    gens.remove(g)

                drive([gla_dir(0), gla_dir(1)])
                P.barrier()

        if upto >= 3:
            with ExitStack() as st:
                def sb(name, shape, dt):
                    return st.enter_context(nc.sbuf_tensor(name, list(shape), dt))

                VALL = sb("VALL", [128, NT, 512], BF16)
                KTh = [sb("KTh%d" % i, [128, T], BF16) for i in range(2)]
                QTh = [sb("QTh%d" % i, [128, S], BF16) for i in range(2)]
                VA = [sb("VA%d" % i, [128, NT, 128], BF16) for i in range(2)]
                r_vall = Res()
                r_kq = [Res(), Res()]
                r_va = [Res(), Res()]
                PTs = Rot([(sb("PTs%d" % i, [128, 512], BF16), Res()) for i in range(3)])
                DEN = Rot([(sb("DEN%d" % i, [128, 512], F32), Res()) for i in range(2)])
                OTs = Rot([(sb("OTs%d" % i, [64, 512], BF16), Res()) for i in range(2)])
                psS = Rot([bank(0), bank(1), bank(2)])
                psO = Rot([bank(4), bank(5)])
                GG = sb("GG", [128, 128], F32)
                r_gg = Res()
                P.dma(lambda e: e.dma_start(out=GG[:], in_=sap(glag_h, 0, [[0, 128], [1, 128]])), writes=[r_gg])
                cOF = [sb("cOF%d" % i, [128, 4, 512], F32) for i in range(2)]
                cOB = [sb("cOB%d" % i, [128, 4, 512], F32) for i in range(2)]
                cGR = [sb("cGR%d" % i, [128, 4, 512], BF16) for i in range(2)]
                cGL = [sb("cGL%d" % i, [128, 4, 512], F32) for i in range(2)]
                cSQ = [sb("cSQ%d" % i, [128, 512], F32) for i in range(2)]
                cSS = [sb("cSS%d" % i, [128, 4], F32) for i in range(2)]
                r_cin = [Res(), Res()]
                r_cgl = [Res(), Res()]
                r_csq = [Res(), Res()]
                r_css = [Res(), Res()]

                def load_c(g):
                    b = g % 2
                    rows = lambda dram: dram.ap()[g * 512:(g + 1) * 512, :].rearrange("(t p) c -> p t c", p=128)
                    P.dma(lambda e: e.dma_start(out=cOF[b][:], in_=rows(OFD)), reads=[R_["OFD"]], writes=[r_cin[b]])
                    P.dma(lambda e: e.dma_start(out=cOB[b][:], in_=rows(OBD)), reads=[R_["OBD"]], pwrites=[r_cin[b]])
                    P.dma(lambda e: e.dma_start(out=cGR[b][:], in_=rows(GRD)), reads=[R_["GRD"]], pwrites=[r_cin[b]])

                def comb_tile(g, t):
                    b = g % 2
                    j = t % 2
                    ow = cGL[b][:, t, :]
                    o3 = sap(cGL[b], t * 512, [[2048, 128], [128, 4], [1, 128]])
                    SQo, SSc = cSQ[j], cSS[j]
                    P.dve(lambda e: e.tensor_tensor(ow, cOF[b][:, t, :], cOB[b][:, t, :], ALU.add), reads=[r_cin[b]], writes=[r_cgl[b]] if t == 0 else (), pwrites=() if t == 0 else [r_cgl[b]])
                    yield
                    P.act(lambda e: e.activation(SQo[:], ow, AF.Square), reads=[r_cgl[b]], writes=[r_csq[j]])
                    yield
                    P.dve(lambda e: e.reduce_sum(SSc[:], sap(SQo, 0, [[512, 128], [128, 4], [1, 128]]), AX.X), reads=[r_csq[j]], writes=[r_css[j]])
                    yield
                    P.dve(lambda e: e.tensor_scalar(SSc[:], SSc[:], 1.0 / 128, EPS, ALU.mult, ALU.add), reads=[r_css[j]], writes=[r_css[j]])
                    yield
                    P.act(lambda e: e.activation(SSc[:], SSc[:], AF.Ln), reads=[r_css[j]], writes=[r_css[j]])
                    yield
                    P.act(lambda e: e.activation(SSc[:], SSc[:], AF.Exp, scale=-0.5), reads=[r_css[j]], writes=[r_css[j]])
                    yield
                    P.dve(lambda e: e.tensor_tensor(o3, o3, sap(SSc, 0, [[4, 128], [1, 4], [0, 128]]), ALU.mult), reads=[r_cgl[b], r_css[j]], pwrites=[r_cgl[b]])
                    yield
                    P.dve(lambda e: e.tensor_tensor(o3, o3, sap(GG, 0, [[128, 128], [0, 4], [1, 128]]), ALU.mult), reads=[r_cgl[b], r_gg], pwrites=[r_cgl[b]])
                    yield
                    P.dve(lambda e: e.tensor_tensor(ow, ow, cGR[b][:, t, :], ALU.mult), reads=[r_cgl[b], r_cin[b]], pwrites=[r_cgl[b]])
                    yield


                def comb_all():
                    load_c(0)
                    yield
                    for g in range(8):
                        if g + 1 < 8:
                            load_c(g + 1)
                        for pr_ in ((0, 1), (2, 3)):
                            gs = [comb_tile(g, pr_[0]), comb_tile(g, pr_[1])]
                            while gs:
                                for x in list(gs):
                                    try:
                                        next(x)
                                    except StopIteration:
                                        gs.remove(x)
                                yield
                        P.dma(lambda e, g=g: e.dma_start(out=GLD.ap()[g * 512:(g + 1) * 512, :].rearrange("(t p) c -> p t c", p=128), in_=cGL[g % 2][:]),
                              reads=[r_cgl[g % 2]], pwrites=[R_["GLD"]])
                        yield

                comb_gen = comb_all()

                def comb_step(n):
                    for _ in range(n):
                        try:
                            next(comb_gen)
                        except StopIteration:
                            return False
                    return True


                P.dma(lambda e: e.dma_start(out=VALL[:], in_=VD.ap().rearrange("(n p) c -> p n c", p=128)), reads=[R_["VD"]], writes=[r_vall])
                for i in range(2):
                    P.dve(lambda e, i=i: e.memset(KTh[i][96:128, :], 0.0), writes=[r_kq[i]])
                    P.dve(lambda e, i=i: e.memset(QTh[i][96:128, :], 0.0), pwrites=[r_kq[i]])
                    P.dve(lambda e, i=i: e.memset(VA[i][:, :, 64:128], 1.0), writes=[r_va[i]])

                def load_head(h):
                    b = h % 2
                    P.dma(lambda e: e.dma_start(out=KTh[b][0:64, :], in_=KNT.ap()[h // 2, (h % 2) * 64:(h % 2) * 64 + 64, :]), reads=[R_["KNT"]], pwrites=[r_kq[b]])
                    P.dma(lambda e: e.dma_start(out=KTh[b][64:96, :], in_=KRT.ap()), reads=[R_["KRT"]], pwrites=[r_kq[b]])
                    P.dma(lambda e: e.dma_start(out=QTh[b][0:96, :], in_=QTD.ap()[h, :, :]), reads=[R_["QTD"]], pwrites=[r_kq[b]])
                    P.dve(lambda e: e.tensor_copy(VA[b][:, :, 0:64], VALL[:, :, h * 64:(h + 1) * 64]), reads=[r_vall], pwrites=[r_va[b]])

                def attend(h, qb):
                    b = h % 2
                    q0 = qb * 512
                    pot, poo, por = psO.next()
                    sc = {}

                    def S_(kt):
                        pt, po, pr = psS.next()
                        sc[kt] = (pt, po, pr)
                        P.pe(lambda e: e.matmul(pt[:, po:po + 512], KTh[b][:, kt * 128:(kt + 1) * 128], QTh[b][:, q0:q0 + 512], start=True, stop=True),
                             reads=[r_kq[b]], writes=[pr])

                    S_(0)
                    S_(1)
                    for kt in range(NT):
                        pt, po, pr = sc.pop(kt)
                        pbuf, rp = PTs.next()
                        P.act(lambda e, pt=pt, po=po, pbuf=pbuf: e.activation(pbuf[:], pt[:, po:po + 512], AF.Exp), reads=[pr], writes=[rp])
                        if kt + 2 < NT:
                            S_(kt + 2)
                        P.pe(lambda e, kt=kt, pbuf=pbuf: e.matmul(pot[:, poo:poo + 512], VA[b][:, kt, :], pbuf[:], start=(kt == 0), stop=(kt == NT - 1)),
                             reads=[r_va[b], rp], writes=[por] if kt == 0 else (), pwrites=() if kt == 0 else [por])
                    den, rden = DEN.next()
                    ot, rot = OTs.next()
                    P.dve(lambda e: e.reciprocal(den[64:128, :], pot[64:128, poo:poo + 512]), reads=[por], writes=[rden])
                    P.dve(lambda e: e.tensor_tensor(ot[0:64, :], pot[0:64, poo:poo + 512], den[64:128, :], ALU.mult), reads=[por, rden], writes=[rot])
                    P.dma(lambda e: e.dma_start(out=MTD.ap()[h * 64:(h + 1) * 64, q0:q0 + 512], in_=ot[0:64, :]), reads=[rot], pwrites=[R_["MTD"]])

                load_head(0)
                for h in range(min(8, DEV_AH)):
                    if h + 1 < 8:
                        load_head(h + 1)
                    for qb in range(min(8, DEV_AQ)):
                        comb_step(3)
                        attend(h, qb)
                while comb_step(8):
                    pass
                P.barrier()

        P.default_q = "sp"

        def ln_core(sbt, T1, rT1, MV, rMV, XN, rXN, dst=None, rdst=None, wdst=None):
            XNa = XN if isinstance(XN, bass.AP) else XN[:]
            P.act(lambda e: e.activation(XNa, T1[:], AF.Square), reads=[rT1], writes=[rXN])
            P.dve(lambda e: e.reduce_sum(MV[:, 0:1], T1[:], AX.X), reads=[rT1], writes=[rMV])
            yield
            P.dve(lambda e: e.reduce_sum(MV[:, 1:2], XNa, AX.X), reads=[rXN], pwrites=[rMV])
            yield
            P.dve(lambda e: e.tensor_scalar_mul(MV[:, 0:2], MV[:, 0:2], 1.0 / D), reads=[rMV], writes=[rMV])
            yield
            P.dve(lambda e: e.tensor_tensor(MV[:, 2:3], MV[:, 0:1], MV[:, 0:1], ALU.mult), reads=[rMV], writes=[rMV])
            yield
            P.dve(lambda e: e.scalar_tensor_tensor(MV[:, 3:4], MV[:, 1:2], EPS, MV[:, 2:3], ALU.add, ALU.subtract), reads=[rMV], writes=[rMV])
            yield
            P.act(lambda e: e.activation(MV[:, 3:4], MV[:, 3:4], AF.Ln), reads=[rMV], writes=[rMV])
            yield
            P.act(lambda e: e.activation(MV[:, 3:4], MV[:, 3:4], AF.Exp, scale=-0.5), reads=[rMV], writes=[rMV])
            yield
            if dst is None:
                P.dve(lambda e: e.tensor_scalar(XNa, T1[:], MV[:, 0:1], MV[:, 3:4], ALU.subtract, ALU.mult), reads=[rT1, rMV, rXN], writes=[rXN])
            else:
                P.dve(lambda e: e.tensor_scalar(dst, T1[:], MV[:, 0:1], MV[:, 3:4], ALU.subtract, ALU.mult), reads=[rT1, rMV], **wdst)
            yield

        if upto >= 4:
            with ExitStack() as st4:
                HT = st4.enter_context(nc.sbuf_tensor("HT", [128, 8, S], BF16))
                COMBT = st4.enter_context(nc.sbuf_tensor("COMBT", [128, S], BF16))
                r_ht, r_combt = Res(), Res()
                P.dve(lambda e: e.memset(COMBT[:], 0.0), writes=[r_combt])
                with ExitStack() as st:
                    def sb(name, shape, dt):
                        return st.enter_context(nc.sbuf_tensor(name, list(shape), dt))

                    WO = sb("WO", [128, 8, D], BF16)
                    LG1 = sb("LG1", [128, D], F32)
                    LB1 = sb("LB1", [128, D], F32)
                    WR = sb("WR", [128, 8, 20], F32)
                    BR = sb("BR", [128, 20], F32)
                    r_c = Res()
                    XTs = [sb("XT%d" % i, [128, 2, D], F32) for i in range(2)]
                    GLt = [sb("GLt%d" % i, [128, 2, 512], F32) for i in range(2)]
                    MIX = [sb("MIX%d" % i, [128, 8, 256], BF16) for i in range(2)]
                    r_xt, r_glt, r_mix = [Res(), Res()], [Res(), Res()], [Res(), Res()]
                    T1s = [sb("T1_%d" % i, [128, D], F32) for i in range(2)]
                    XN = sb("XN", [128, D], F32)
                    X1 = [sb("X1_%d" % i, [128, 2, D], F32) for i in range(2)]
                    HHs = [sb("HH%d" % i, [128, D], F32) for i in range(2)]
                    HTfs = [sb("HTf%d" % i, [128, 8, 128], F32) for i in range(2)]
                    MVs = [sb("MV%d" % i, [128, 8], F32) for i in range(2)]
                    RTs = [sb("RT%d" % i, [128, 96], F32) for i in range(2)]
                    r_t1s, r_xn, r_x1, r_hhs, r_htfs, r_mvs, r_rts = [Res(), Res()], Res(), [[Res(), Res()], [Res(), Res()]], [Res(), Res()], [Res(), Res()], [Res(), Res()], [Res(), Res()]
                    r_ps3 = Res()
                    r_b7 = [[r_ps3, r_ps3], [r_ps3, r_ps3]]
                    wo_v = wo_h.ap().rearrange("(k p) n -> p k n", p=128)
                    for k in range(8):
                        ws, rws = XTs[k % 2], r_xt[k % 2]
                        P.dma(lambda e, k=k, ws=ws: e.dma_start(out=ws[:, 0, :], in_=wo_v[:, k, :]), writes=[rws])
                        if k % 2 == 0:
                            P.dve(lambda e, k=k, ws=ws: e.tensor_copy(WO[:, k, :], ws[:, 0, :]), reads=[rws], pwrites=[r_c])
                        else:
                            P.act(lambda e, k=k, ws=ws: e.copy(WO[:, k, :], ws[:, 0, :]), reads=[rws], pwrites=[r_c])
                    P.dma(lambda e: e.dma_start(out=LG1[:], in_=sap(ln1g_h, 0, [[0, 128], [1, D]])), pwrites=[r_c])
                    P.dma(lambda e: e.dma_start(out=LB1[:], in_=sap(ln1b_h, 0, [[0, 128], [1, D]])), pwrites=[r_c])
                    P.dma(lambda e: e.dma_start(out=WR[:], in_=wr_h.ap().rearrange("(k p) n -> p k n", p=128)), pwrites=[r_c])
                    P.dma(lambda e: e.dma_start(out=BR[:], in_=sap(br_h, 0, [[0, 128], [1, 20]])), pwrites=[r_c])

                    def load_a(g):
                        b = g % 2
                        s0 = g * 256
                        P.dma(lambda e: e.dma_start(out=XTs[b][:], in_=x_h.ap()[s0:s0 + 256, :].rearrange("(t p) d -> p t d", p=128)), writes=[r_xt[b]])
                        P.dma(lambda e: e.dma_start(out=GLt[b][:], in_=GLD.ap()[s0:s0 + 256, :].rearrange("(t p) d -> p t d", p=128)), reads=[R_["GLD"]], writes=[r_glt[b]])
                        P.dma(lambda e: e.dma_start(out=MIX[b][:, 4:8, :], in_=MTD.ap().rearrange("(k p) t -> p k t", p=128)[:, :, s0:s0 + 256]),
                              reads=[R_["MTD"]], writes=[r_mix[b]])

                    def tile_a(tt):
                        b = (tt // 2) % 2
                        tl = tt % 2
                        s0 = tt * 128
                        x1h, rx1 = X1[b], r_x1[b][tl]
                        x1 = x1h[:, tl, :]
                        xt_t = XTs[b][:, tl, :]
                        T1, r_t1 = T1s[tl], r_t1s[tl]
                        HH, r_hh = HHs[tl], r_hhs[tl]
                        HTf, r_htf = HTfs[tl], r_htfs[tl]
                        MV, r_mv = MVs[tl], r_mvs[tl]
                        RT, r_rt = RTs[tl], r_rts[tl]
                        pt, po, pr = bank(6)
                        pr = r_ps3
                        for k in range(4):
                            P.pe(lambda e, k=k: e.transpose(pt[:, po + k * 128:po + (k + 1) * 128], GLt[b][:, tl, k * 128:(k + 1) * 128], ident_f[:]),
                                 reads=[r_glt[b], r_ident], writes=[pr] if k == 0 else (), pwrites=() if k == 0 else [pr])
                        P.act(lambda e: e.copy(sap(MIX[b], tl * 128, [[2048, 128], [256, 4], [1, 128]]), sap(pt, po, [[1024, 128], [128, 4], [1, 128]])), reads=[pr], pwrites=[r_mix[b]])
                        yield
                        PY, rpy = PSt[tl], PSr[tl][0]
                        for nh in range(2):
                            for k in range(8):
                                first = (nh == 0 and k == 0)
                                P.pe(lambda e, nh=nh, k=k: e.matmul(PY[:, nh * 512:(nh + 1) * 512], MIX[b][:, k, tl * 128:(tl + 1) * 128], WO[:, k, nh * 512:(nh + 1) * 512], start=(k == 0), stop=(k == 7)),
                                     reads=[r_mix[b], r_c], writes=[rpy] if first else (), pwrites=() if first else [rpy])
                        yield
                        P.dve(lambda e: e.tensor_tensor(T1[:], PY[:], MODL[:, 2048:3072], ALU.mult), reads=[rpy, r_MODL], writes=[r_t1])
                        yield
                        P.dve(lambda e: e.scalar_tensor_tensor(T1[:], xt_t, ALPHA, T1[:], ALU.mult, ALU.add), reads=[r_xt[b], r_t1], writes=[r_t1])
                        yield
                        yield from ln_core(None, T1, r_t1, MV, r_mv, x1, rx1, dst=x1, rdst=rx1, wdst=dict(writes=[rx1]))
                        P.dve(lambda e: e.tensor_tensor(x1, x1, LG1[:], ALU.mult), reads=[rx1, r_c], writes=[rx1])
                        yield
                        P.dve(lambda e: e.tensor_tensor(x1, x1, LB1[:], ALU.add), reads=[rx1, r_c], writes=[rx1])
                        yield
                        if tl == 1:
                            g0 = (tt // 2) * 256
                            P.dma(lambda e: e.dma_start(out=X1D.ap()[g0:g0 + 256, :].rearrange("(t p) d -> p t d", p=128), in_=x1h[:]), reads=r_x1[b], pwrites=[R_["X1D"]])
                        P.dve(lambda e: e.tensor_tensor(HH[:], x1, MODL[:, 4096:5120], ALU.mult), reads=[rx1, r_MODL], writes=[r_hh])
                        yield
                        P.dve(lambda e: e.tensor_tensor(HH[:], HH[:], MODL[:, 3072:4096], ALU.add), reads=[r_hh, r_MODL], writes=[r_hh])
                        yield
                        PH, rph = PSt[2], PSr[2][0]
                        for k in range(8):
                            P.pe(lambda e, k=k: e.transpose(PH[:, k * 128:(k + 1) * 128], HH[:, k * 128:(k + 1) * 128], ident_f[:]),
                                 reads=[r_hh, r_ident], writes=[rph] if k == 0 else (), pwrites=() if k == 0 else [rph])
                        P.act(lambda e: e.copy(sap(HTf, 0, [[1024, 128], [1, 1024]]), PH[:]), reads=[rph], writes=[r_htf])
                        yield
                        P.act(lambda e: e.copy(HT[:, :, s0:s0 + 128], HTf[:]), reads=[r_htf], pwrites=[r_ht])
                        pt2, po2_, _ = bank(7)
                        po2 = po2_ + tl * 256
                        pr2, pr3 = r_b7[tl]
                        for k in range(8):
                            P.pe(lambda e, k=k: e.matmul(pt2[:, po2:po2 + 20], HTf[:, k, :], WR[:, k, :], start=(k == 0), stop=(k == 7)),
                                 reads=[r_htf, r_c], writes=[pr2] if k == 0 else (), pwrites=() if k == 0 else [pr2])
                        yield
                        LOG = RT[:, 0:20]; GMX = RT[:, 20:21]; G1H = RT[:, 24:28]; GE = RT[:, 28:32]; PG_ = RT[:, 32:33]
                        MSK = RT[:, 36:52]; EIN = RT[:, 52:56]; M1 = RT[:, 56:57]; MK1 = RT[:, 60:64]; E2 = RT[:, 64:68]
                        M2 = RT[:, 57:58]; MK2 = RT[:, 68:72]; RR = RT[:, 58:59]; W1 = RT[:, 59:60]; W2 = RT[:, 72:73]; CW = RT[:, 76:80]
                        CMB = RT[:, 80:96]
                        rr_ = dict(reads=[r_rt], writes=[r_rt])
                        P.dve(lambda e: e.tensor_tensor(LOG, pt2[:, po2:po2 + 20], BR[:], ALU.add), reads=[pr2, r_c], writes=[r_rt])
                        yield
                        P.dve(lambda e: e.reduce_max(GMX, RT[:, 0:4], AX.X), **rr_)
                        yield
                        P.dve(lambda e: e.tensor_scalar(G1H, RT[:, 0:4], GMX, None, ALU.is_equal), **rr_)
                        P.dve(lambda e: e.tensor_scalar(GE, RT[:, 0:4], GMX, None, ALU.subtract), **rr_)
                        yield
                        P.act(lambda e: e.activation(GE, GE, AF.Exp), **rr_)
                        yield
                        P.dve(lambda e: e.reduce_sum(PG_, GE, AX.X), **rr_)
                        yield
                        P.dve(lambda e: e.reciprocal(PG_, PG_), **rr_)
                        yield
                        P.dve(lambda e: e.tensor_tensor(sap(RT, 36, [[96, 128], [4, 4], [1, 4]]), sap(RT, 4, [[96, 128], [4, 4], [1, 4]]),
                                                        sap(RT, 24, [[96, 128], [1, 4], [0, 4]]), ALU.mult), **rr_)
                        yield
                        P.dve(lambda e: e.reduce_sum(EIN, sap(RT, 36, [[96, 128], [1, 4], [4, 4]]), AX.X), **rr_)
                        yield
                        P.dve(lambda e: e.reduce_max(M1, EIN, AX.X), **rr_)
                        yield
                        P.dve(lambda e: e.tensor_scalar(MK1, EIN, M1, None, ALU.is_equal), **rr_)
                        yield
                        P.dve(lambda e: e.scalar_tensor_tensor(E2, MK1, -1e30, EIN, ALU.mult, ALU.add), **rr_)
                        yield
                        P.dve(lambda e: e.reduce_max(M2, E2, AX.X), **rr_)
                        yield
                        P.dve(lambda e: e.tensor_scalar(MK2, E2, M2, None, ALU.is_equal), **rr_)
                        P.dve(lambda e: e.tensor_tensor(RR, M2, M1, ALU.subtract), **rr_)
                        yield
                        P.act(lambda e: e.activation(RR, RR, AF.Exp), **rr_)
                        yield
                        P.dve(lambda e: e.tensor_scalar_add(W1, RR, 1.0), **rr_)
                        yield
                        P.dve(lambda e: e.reciprocal(W1, W1), **rr_)
                        yield
                        P.dve(lambda e: e.tensor_tensor(W1, W1, PG_, ALU.mult), **rr_)
                        yield
                        P.dve(lambda e: e.tensor_tensor(W2, W1, RR, ALU.mult), **rr_)
                        P.dve(lambda e: e.tensor_scalar(CW, MK1, W1, None, ALU.mult), **rr_)
                        yield
                        P.dve(lambda e: e.scalar_tensor_tensor(CW, MK2, W2, CW, ALU.mult, ALU.add), **rr_)
                        yield
                        P.dve(lambda e: e.tensor_tensor(sap(RT, 80, [[96, 128], [4, 4], [1, 4]]), sap(RT, 24, [[96, 128], [1, 4], [0, 4]]),
                                                        sap(RT, 76, [[96, 128], [0, 4], [1, 4]]), ALU.mult), **rr_)
                        yield
                        po3 = po2_ + 128 + tl * 256
                        P.pe(lambda e: e.transpose(pt2[0:16, po3:po3 + 128], CMB, ident_f[:]), reads=[r_rt, r_ident], writes=[pr3])
                        P.dve(lambda e: e.tensor_copy(COMBT[0:16, s0:s0 + 128], pt2[0:16, po3:po3 + 128]), reads=[pr3], pwrites=[r_combt])
                        if debug and DEV_DBGC:
                            P.dma(lambda e: e.dma_start(out=dbg_comb.ap()[s0:s0 + 128, :], in_=CMB), reads=[r_rt])
                        yield

                    if debug:
                        dbg_comb = nc.dram_tensor("dbg_comb", [S, 16], F32, kind="ExternalOutput")
                    load_a(0)
                    for g in range(16):
                        if g + 1 < 16:
                            load_a(g + 1)
                        drive([tile_a(2 * g), tile_a(2 * g + 1)])
                    P.barrier()

                if upto >= 5:
                    with ExitStack() as st:
                        def sb(name, shape, dt):
                            return st.enter_context(nc.sbuf_tensor(name, list(shape), dt))

                        SELi = sb("SELi", [128, 16, 128], I32)
                        SELf = sb("SELf", [128, 16, 128], F32)
                        SEL = sb("SEL", [128, 16, 128], BF16)
                        r_sel = Res()
                        P.pool(lambda e: e.iota(SELi[:], pattern=[[1, 16], [0, 128]], base=0, channel_multiplier=-1), writes=[r_sel])
                        P.dve(lambda e: e.tensor_copy(SELf[:], SELi[:]), reads=[r_sel], writes=[r_sel])
                        P.dve(lambda e: e.tensor_single_scalar(SEL[:], SELf[:], 0.0, ALU.is_equal), reads=[r_sel], writes=[r_sel])
                        WST = Rot([(sb("WS4_%d" % i, [128, 8, 256], F32), Res()) for i in range(2)])
                        WG = [sb("WG%d" % i, [128, 8, 256], BF16) for i in range(2)]
                        WU = [sb("WU%d" % i, [128, 8, 256], BF16) for i in range(2)]
                        r_wg = [Res(), Res()]
                        SG = Rot([(sb("SG%d" % i, [128, 512], BF16), Res()) for i in range(2)])
                        TT = Rot([(sb("TT%d" % i, [128, 512], BF16), Res()) for i in range(2)])
                        HEs = Rot([(sb("HEs%d" % i, [128, 2, 2048], BF16), Res()) for i in range(DEV_NHE)])
                        he_cur = [None]
                        psG = Rot([bank(0), bank(1)])
                        psU = Rot([bank(2), bank(3)])
                        psC = Rot([bank(4), bank(5)])

                        def load_w(ex):
                            b = ex % 2
                            for j, (wh, dst) in enumerate(((weg_h, WG[b]), (weu_h, WU[b]))):
                                ws, rws = WST.next()
                                P.dma(lambda e, ws=ws, wh=wh: e.dma_start(out=ws[:], in_=wh.ap()[ex].rearrange("(k p) f -> p k f", p=128)), writes=[rws])
                                wr = dict(writes=[r_wg[b]]) if j == 0 else dict(pwrites=[r_wg[b]])
                                if j == 0:
                                    P.dve(lambda e, ws=ws, dst=dst: e.tensor_copy(dst[:], ws[:]), reads=[rws], **wr)
                                else:
                                    P.act(lambda e, ws=ws, dst=dst: e.copy(dst[:], ws[:]), reads=[rws], **wr)

                        def expert_blk(ex, blk):
                            b = ex % 2
                            c0 = blk * 512
                            pct, pco, pcr = psC.next()
                            P.pe(lambda e: e.matmul(pct[:, pco:pco + 512], SEL[:, ex, :], COMBT[:, c0:c0 + 512], start=True, stop=True),
                                 reads=[r_sel, r_combt], writes=[pcr])
                            if blk % 4 == 0:
                                he_cur[0] = HEs.next()
                            he, rhe = he_cur[0]
                            hc = (blk % 4) * 512
                            for mc in range(2):
                                pgt, pgo, pgr = psG.next()
                                put, puo, pur = psU.next()
                                for k in range(8):
                                    P.pe(lambda e, k=k, mc=mc, pgt=pgt, pgo=pgo: e.matmul(pgt[:, pgo:pgo + 512], WG[b][:, k, mc * 128:(mc + 1) * 128], HT[:, k, c0:c0 + 512], start=(k == 0), stop=(k == 7)),
                                         reads=[r_wg[b], r_ht], writes=[pgr] if k == 0 else (), pwrites=() if k == 0 else [pgr])
                                for k in range(8):
                                    P.pe(lambda e, k=k, mc=mc, put=put, puo=puo: e.matmul(put[:, puo:puo + 512], WU[b][:, k, mc * 128:(mc + 1) * 128], HT[:, k, c0:c0 + 512], start=(k == 0), stop=(k == 7)),
                                         reads=[r_wg[b], r_ht], writes=[pur] if k == 0 else (), pwrites=() if k == 0 else [pur])
                                if DEV_B < 3:
                                    continue
                                sg, rsg = SG.next()
                                tt_, rtt = TT.next()
                                P.act(lambda e, sg=sg, pgt=pgt, pgo=pgo: e.activation(sg[:], pgt[:, pgo:pgo + 512], AF.Silu), reads=[pgr], writes=[rsg])
                                P.dve(lambda e, sg=sg, tt_=tt_, put=put, puo=puo: e.tensor_tensor(tt_[:], put[:, puo:puo + 512], sg[:], ALU.mult), reads=[pur, rsg], writes=[rtt])
                                first = (mc == 0 and blk % 4 == 0)
                                P.dve(lambda e, tt_=tt_, mc=mc: e.tensor_tensor(he[:, mc, hc:hc + 512], pct[:, pco:pco + 512], tt_[:], ALU.mult), reads=[pcr, rtt],
                                      writes=[rhe] if first else (), pwrites=() if first else [rhe])
                            if DEV_B >= 4 and blk % 4 == 3:
                                g0 = (blk // 4) * 2048
                                P.dma(lambda e: e.dma_start(out=HED.ap()[ex if DEV_HEDX else 0].rearrange("(m p) t -> p m t", p=128)[:, :, g0:g0 + 2048], in_=he[:]), reads=[rhe], pwrites=[R_["HED"]], q=DEV_HQ)

                        load_w(0)
                        for ex in range(min(NE, DEV_BE)):
                            if ex + 1 < NE:
                                load_w(ex + 1)
                            for blk in range(8 if DEV_B >= 2 else 0):
                                expert_blk(ex, blk)
                        P.barrier()
            P.barrier()
        if upto >= 6:
            with ExitStack() as st:
                def sb(name, shape, dt):
                    return st.enter_context(nc.sbuf_tensor(name, list(shape), dt))

                WD = sb("WD", [128, 32, D], BF16)
                LG2 = sb("LG2", [128, D], F32)
                LB2 = sb("LB2", [128, D], F32)
                r_cc = Res()
                HEb = [sb("HEb%d" % i, [128, 32, 512], BF16) for i in range(2)]
                r_heb = [Res(), Res()]
                X1t = [sb("X1t%d" % i, [128, 2, D], F32) for i in range(2)]
                r_x1t = [Res(), Res()]
                T1c = sb("T1c", [128, D], F32); XNc = sb("XNc", [128, D], F32)
                OUTt = [sb("OUTt%d" % i, [128, 2, D], F32) for i in range(2)]
                r_out = [Res(), Res()]
                MVc = sb("MVc", [128, 8], F32)
                r_t1c, r_xnc, r_mvc = Res(), Res(), Res()
                wd_v = wed_h.ap().rearrange("e (m p) n -> p (e m) n", p=128)
                r_wd = [Res() for _ in range(16)]
                for e2 in range(16):
                    ws, rws = X1t[e2 % 2], r_x1t[e2 % 2]
                    P.dma(lambda e, e2=e2, ws=ws: e.dma_start(out=ws[:], in_=wd_v[:, 2 * e2:2 * e2 + 2, :]), writes=[rws])
                    if e2 % 2 == 0:
                        P.dve(lambda e, e2=e2, ws=ws: e.tensor_copy(WD[:, 2 * e2:2 * e2 + 2, :], ws[:]), reads=[rws], writes=[r_wd[e2]])
                    else:
                        P.act(lambda e, e2=e2, ws=ws: e.copy(WD[:, 2 * e2:2 * e2 + 2, :], ws[:]), reads=[rws], writes=[r_wd[e2]])
                P.dma(lambda e: e.dma_start(out=LG2[:], in_=sap(ln2g_h, 0, [[0, 128], [1, D]])), pwrites=[r_cc])
                P.dma(lambda e: e.dma_start(out=LB2[:], in_=sap(ln2b_h, 0, [[0, 128], [1, D]])), pwrites=[r_cc])
                hed_v = HED.ap().rearrange("e (m p) t -> p (e m) t", p=128)

                def load_he(blk):
                    b = blk % 2
                    c0 = blk * 512
                    P.dma(lambda e: e.dma_start(out=HEb[b][:], in_=hed_v[:, :, c0:c0 + 512]), reads=[R_["HED"]], writes=[r_heb[b]])

                def load_x1(g):
                    b = g % 2
                    P.dma(lambda e: e.dma_start(out=X1t[b][:], in_=X1D.ap()[g * 256:(g + 1) * 256, :].rearrange("(t p) d -> p t d", p=128)), reads=[R_["X1D"]], writes=[r_x1t[b]])

                def tile_c(tt):
                    blk, t = tt // 4, tt % 4
                    hb, rhb = HEb[blk % 2], r_heb[blk % 2]
                    b = (tt // 2) % 2
                    tl = tt % 2
                    PY, rpy = PSt[tt % 2], PSr[tt % 2][0]
                    for nh in range(2):
                        for em in range(32):
                            first = (nh == 0 and em == 0)
                            P.pe(lambda e, nh=nh, em=em: e.matmul(PY[:, nh * 512:(nh + 1) * 512], hb[:, em, t * 128:(t + 1) * 128], WD[:, em, nh * 512:(nh + 1) * 512], start=(em == 0), stop=(em == 31)),
                                 reads=[rhb, r_wd[em // 2]], writes=[rpy] if first else (), pwrites=() if first else [rpy])
                    oth, rot = OUTt[b], r_out[b]
                    ot = oth[:, tl, :]
                    P.dve(lambda e: e.tensor_tensor(T1c[:], PY[:], MODL[:, 5120:6144], ALU.mult), reads=[rpy, r_MODL], writes=[r_t1c])
                    P.dve(lambda e: e.scalar_tensor_tensor(T1c[:], X1t[b][:, tl, :], ALPHA, T1c[:], ALU.mult, ALU.add), reads=[r_x1t[b], r_t1c], writes=[r_t1c])
                    drive([ln_core(None, T1c, r_t1c, MVc, r_mvc, XNc, r_xnc)])
                    wo_ = dict(writes=[rot]) if tl == 0 else dict(pwrites=[rot])
                    P.dve(lambda e: e.tensor_tensor(ot, XNc[:], LG2[:], ALU.mult), reads=[r_xnc, r_cc], **wo_)
                    P.dve(lambda e: e.tensor_tensor(ot, ot, LB2[:], ALU.add), reads=[rot, r_cc], pwrites=[rot])
                    if tl == 1:
                        g0 = (tt // 2) * 256
                        P.dma(lambda e: e.dma_start(out=out_h.ap()[g0:g0 + 256, :].rearrange("(t p) d -> p t d", p=128), in_=oth[:]), reads=[rot], pwrites=[R_["OUT"]])

                load_he(0)
                load_x1(0)
                for tt in range(32):
                    if tt % 4 == 0 and tt // 4 + 1 < 8:
                        load_he(tt // 4 + 1)
                    if tt % 2 == 0 and tt // 2 + 1 < 16:
                        load_x1(tt // 2 + 1)
                    tile_c(tt)
                P.barrier()

        P.barrier()
        finals = [op for op in P.all if op.dma]
        P.emit(final_ops=finals)
    return nc, P


def make_in_maps(inputs):
    f = lambda a: np.ascontiguousarray(np.asarray(a, dtype=np.float32))
    x = f(inputs["x"]); c = f(inputs["c"]); ctx = f(inputs["ctx"])
    wgk = np.zeros((33, 512), np.float32)
    wgk[0:16, 0:256] = inputs["w_gk_f"][0]
    wgk[16:32, 256:512] = inputs["w_gk_b"][0]
    wgk[32, 0:256] = inputs["b_gk_f"][0]
    wgk[32, 256:512] = inputs["b_gk_b"][0]
    shared = {
        "c_ctx": f(inputs["c_ctx"]),
        "w_ada": f(inputs["w_ada"][0]), "b_ada": f(inputs["b_ada"][0]),
        "w_in": f(inputs["w_in"][0]), "wgk": wgk,
        "gla_g": f(inputs["gla_norm_g"][0]), "q_g": f(inputs["mla_q_norm_g"][0]),
        "w_uq": f(inputs["w_uq"][0]), "kv_g": f(inputs["mla_kv_norm_g"][0]),
        "w_ukv": f(inputs["w_ukv"][0]), "w_o": f(inputs["w_o"][0]),
        "ln1_g": f(inputs["ln1_g"][0]), "ln1_b": f(inputs["ln1_b"][0]),
        "ln2_g": f(inputs["ln2_g"][0]), "ln2_b": f(inputs["ln2_b"][0]),
        "w_r": f(np.concatenate([inputs["w_router_group"][0], inputs["w_router_expert"][0]], axis=1)),
        "b_r": f(np.concatenate([inputs["b_router_group"][0], inputs["b_router_expert"][0]], axis=0)),
        "w_eg": f(inputs["w_expert_gate"][0]), "w_eu": f(inputs["w_expert_up"][0]),
        "w_ed": f(inputs["w_expert_down"][0]),
    }
    maps = []
    for b in range(8):
        m = dict(shared)
        m["x"] = x[b]
        m["ctx"] = ctx[b]
        m["c"] = c[b]
        maps.append(m)
    return maps


_CACHE = {}


def kernel(**inputs):
    if "nc" not in _CACHE:
        _CACHE["nc"] = build()[0]
    nc = _CACHE["nc"]
    in_maps = make_in_maps(inputs)
    res = run_bass_kernel_spmd(nc, in_maps, core_ids=list(range(8)))
    return np.stack([np.asarray(r["out"]).reshape(S, D) for r in res.results], axis=0).astype(np.float32)
```

```python
import math
from contextlib import ExitStack

import numpy as np
import concourse.bass as bass
import concourse.mybir as mybir
from concourse.bass_utils import run_bass_kernel_spmd

F32 = mybir.dt.float32
BF16 = mybir.dt.bfloat16
I32 = mybir.dt.int32
ALU = mybir.AluOpType
AF = mybir.ActivationFunctionType
AX = mybir.AxisListType

ENGS = ("pe", "act", "dve", "pool", "sp")

D = 1024
S = 4096
CT = 256
T = S + CT
NT = T // 128
EPS = 1e-6
ALPHA = 2.0 ** 0.25
NE = 16
DEV_NBLK = 99
DEV_STAGE = 99
DEV_G = 3
DEV_HQ = "pool"
DEV_NHE = 2
DEV_HEDX = True
DEV_DBGC = False
DEV_B = 4
DEV_BE = 16
DEV_A4 = 32
DEV_AH = 8
DEV_AQ = 8
DEV_GA = 2
DEV_GN = 99
DEV_RQ = True


class Res:
    __slots__ = ("name", "w", "ws", "rs")

    def __init__(self, name=""):
        self.name = name
        self.w = None
        self.ws = []
        self.rs = []


class Op:
    __slots__ = ("eng", "fn", "deps", "dma", "sig", "sem", "val", "prev_dma")

    def __init__(self, eng, fn, dma):
        self.eng = eng
        self.fn = fn
        self.deps = []
        self.dma = dma
        self.sig = dma
        self.sem = None
        self.val = 0
        self.prev_dma = None


class Prog:
    def __init__(self, nc, n_dma_sems=28):
        self.nc = nc
        self.ops = {e: [] for e in ENGS}
        self.all = []
        self.n_dma_sems = n_dma_sems
        self.pending = {e: [] for e in ENGS}
        self.dma_since_barrier = []
        self.default_q = "sp"

    def add(self, eng, fn, reads=(), writes=(), pwrites=(), dma=False):
        op = Op(eng, fn, dma)
        deps = []
        seen = set()

        def dep(d):
            if d is not None and id(d) not in seen:
                seen.add(id(d))
                deps.append(d)

        for r in reads:
            dep(r.w)
            for w in r.ws:
                dep(w)
        for w in writes:
            dep(w.w)
            for x in w.ws:
                dep(x)
            for rd in w.rs:
                dep(rd)
        for w in pwrites:
            dep(w.w)
            for rd in w.rs:
                dep(rd)
        for d in self.pending[eng]:
            dep(d)
        self.pending[eng] = []
        for d in deps:
            d.sig = True
            op.deps.append(d)
        for r in reads:
            r.rs.append(op)
        for w in writes:
            w.w = op
            w.ws = []
            w.rs = []
        for w in pwrites:
            w.ws.append(op)
        self.ops[eng].append(op)
        self.all.append(op)
        if dma:
            self.dma_since_barrier.append(op)
        return op

    def pe(self, fn, reads=(), writes=(), pwrites=()):
        return self.add("pe", fn, reads, writes, pwrites)

    def act(self, fn, reads=(), writes=(), pwrites=()):
        return self.add("act", fn, reads, writes, pwrites)

    def dve(self, fn, reads=(), writes=(), pwrites=()):
        return self.add("dve", fn, reads, writes, pwrites)

    def pool(self, fn, reads=(), writes=(), pwrites=()):
        return self.add("pool", fn, reads, writes, pwrites)

    def dma(self, fn, reads=(), writes=(), pwrites=(), q=None):
        return self.add(q or self.default_q, fn, reads, writes, pwrites, dma=True)

    def barrier(self):
        deps = list(self.dma_since_barrier)
        for e in ENGS:
            for op in reversed(self.ops[e]):
                if not op.dma:
                    deps.append(op)
                    break
        self.dma_since_barrier = []
        for e in ENGS:
            self.pending[e] = list(self.pending[e]) + deps

    def emit(self, final_ops=()):
        nc = self.nc
        with ExitStack() as st:
            esem = {e: st.enter_context(nc.semaphore("s_" + e)) for e in ENGS}
            dsems = {
                q: [st.enter_context(nc.semaphore("d_%s_%d" % (q, i))) for i in range(self.n_dma_sems)]
                for q in ("sp", "act", "pool")
            }
            cnt = {e: 0 for e in ENGS}
            dcnt = {q: 0 for q in dsems}
            duse = {q: [0] * self.n_dma_sems for q in dsems}
            dlast = {q: [None] * self.n_dma_sems for q in dsems}
            for op in self.all:
                if op.dma:
                    q = op.eng
                    k = dcnt[q] % self.n_dma_sems
                    dcnt[q] += 1
                    duse[q][k] += 1
                    op.sem = dsems[q][k]
                    op.val = 16 * duse[q][k]
                    op.prev_dma = dlast[q][k]
                    dlast[q][k] = op
                elif op.sig:
                    cnt[op.eng] += 1
                    op.sem = esem[op.eng]
                    op.val = cnt[op.eng]
            self.stats = dict(cnt=cnt, dcnt=dcnt, nops={e: len(v) for e, v in self.ops.items()})
            finals = list(final_ops)
            with nc.Block() as block:

                def stream(eng_name):
                    def body(eng):
                        seen = {}
                        nwait = 0
                        for op in self.ops[eng_name]:
                            dl = list(op.deps)
                            if op.dma and op.prev_dma is not None:
                                dl.append(op.prev_dma)
                            for d in dl:
                                key = id(d.sem)
                                if seen.get(key, 0) >= d.val:
                                    continue
                                seen[key] = d.val
                                eng.wait_ge(d.sem, d.val)
                                nwait += 1
                            ins = op.fn(eng)
                            if op.dma:
                                ins.then_inc(op.sem, 16)
                            elif op.sig:
                                ins.then_inc(op.sem, 1)
                        if eng_name == "sp":
                            for d in finals:
                                key = id(d.sem)
                                if seen.get(key, 0) >= d.val:
                                    continue
                                seen[key] = d.val
                                eng.wait_ge(d.sem, d.val)
                        self.stats["wait_" + eng_name] = nwait

                    return body

                block.tensor(stream("pe"))
                block.scalar(stream("act"))
                block.vector(stream("dve"))
                block.gpsimd(stream("pool"))
                block.sync(stream("sp"))


class Rot:
    def __init__(self, items):
        self.items = items
        self.i = 0

    def next(self):
        it = self.items[self.i % len(self.items)]
        self.i += 1
        return it


def sap(h, off, dims):
    return bass.AP(h, off, [list(d) for d in dims])


def build(debug=False, upto=99):
    nc = bass.Bass("TRN2", target_bir_lowering=False)
    P = Prog(nc)
    dbg_kind = "ExternalOutput" if debug else "Internal"

    def din(name, shape):
        return nc.dram_tensor(name, list(shape), F32, kind="ExternalInput")

    x_h = din("x", [S, D])
    ctx_h = din("ctx", [CT, D])
    c_h = din("c", [D])
    cctx_h = din("c_ctx", [D])
    wada_h = din("w_ada", [D, 6 * D])
    bada_h = din("b_ada", [6 * D])
    win_h = din("w_in", [D, 1984])
    wgk_h = din("wgk", [33, 512])
    glag_h = din("gla_g", [128])
    qg_h = din("q_g", [256])
    wuq_h = din("w_uq", [256, 768])
    kvg_h = din("kv_g", [128])
    wukv_h = din("w_ukv", [128, 1024])
    wo_h = din("w_o", [D, D])
    ln1g_h = din("ln1_g", [D])
    ln1b_h = din("ln1_b", [D])
    ln2g_h = din("ln2_g", [D])
    ln2b_h = din("ln2_b", [D])
    wr_h = din("w_r", [D, 20])
    br_h = din("b_r", [20])
    weg_h = din("w_eg", [NE, D, 256])
    weu_h = din("w_eu", [NE, D, 256])
    wed_h = din("w_ed", [NE, 256, D])
    out_h = nc.dram_tensor("out", [S, D], F32, kind="ExternalOutput")

    def scratch(name, shape, dt):
        return nc.dram_tensor(name, list(shape), dt, kind=dbg_kind)

    GQT = scratch("GQT", [256, T], BF16)
    GKT = scratch("GKT", [256, T], BF16)
    GKD = scratch("GKD", [T, 256], BF16)
    GVD = scratch("GVD", [T, 512], BF16)
    GRD = scratch("GRD", [S, 512], BF16)
    SPD = scratch("SPD", [T, 512], BF16)
    QTD = scratch("QTD", [8, 96, S], BF16)
    KNT = scratch("KNT", [4, 128, T], BF16)
    KRT = scratch("KRT", [32, T], BF16)
    VD = scratch("VD", [T, 512], BF16)
    OFD = scratch("OFD", [S, 512], F32)
    GLD = scratch("GLD", [S, 512], F32)
    MTD = scratch("MTD", [512, S], BF16)
    X1D = scratch("X1D", [S, D], F32)
    HED = scratch("HED", [NE, 256, S], BF16)
    R_ = {k: Res(k) for k in "GQT GKT GKD GVD GRD SPD QTD KNT KRT VD OFD GLD MTD X1D HED OUT".split()}

    with ExitStack() as gst:
        def gsb(name, shape, dt):
            return gst.enter_context(nc.sbuf_tensor(name, list(shape), dt))

        PSt = [gst.enter_context(nc.psum_tensor("ps%d" % i, [128, 1024], F32)) for i in range(4)]
        PSr = [[Res("ps%d_%d" % (i, j)) for j in range(2)] for i in range(4)]

        def bank(b):
            return PSt[b // 2], (b % 2) * 512, PSr[b // 2][b % 2]

        ident_f = gsb("ident_f", [128, 128], F32)
        ident_b = gsb("ident_b", [128, 128], BF16)
        ones_f = gsb("ones_f", [128, 128], F32)
        MODL = gsb("MODL", [128, 6 * D], F32)
        FM = gsb("FM", [128, 32], F32)
        r_ident = Res("ident")
        r_MODL = Res("MODL")
        r_FM = Res("FM")
        mk = {}
        diff = gsb("diff", [128, 128], F32)
        for _n in ("cum_f", "cum_b", "rem_f", "rem_b", "msk_f", "msk_b"):
            mk[_n] = gsb("mk_" + _n, [128, 128], BF16)

        with ExitStack() as st:
            def sb(name, shape, dt):
                return st.enter_context(nc.sbuf_tensor(name, list(shape), dt))

            io_i = sb("io_i", [128, 128], I32)
            r_diff = Res("diff")
            r_io = Res("io")
            P.pool(lambda e: e.iota(io_i[:], pattern=[[1, 128]], base=0, channel_multiplier=-1), writes=[r_io])
            P.dve(lambda e: e.tensor_copy(diff[:], io_i[:]), reads=[r_io], writes=[r_diff])
            P.dve(lambda e: e.tensor_single_scalar(ident_f[:], diff[:], 0.0, ALU.is_equal), reads=[r_diff], writes=[r_ident])
            P.dve(lambda e: e.tensor_copy(ident_b[:], ident_f[:]), reads=[r_ident], writes=[r_ident])
            P.dve(lambda e: e.memset(ones_f[:], 1.0), writes=[r_ident])
            for name, op, val in (("cum_f", ALU.is_ge, -1.0 / 16), ("cum_b", ALU.is_le, -1.0 / 16),
                                  ("rem_f", ALU.is_lt, -1.0 / 16), ("rem_b", ALU.is_gt, -1.0 / 16),
                                  ("msk_f", ALU.is_ge, 1.0), ("msk_b", ALU.is_le, 1.0)):
                m = mk[name]
                P.dve(lambda e, m=m, op=op, val=val: e.tensor_scalar(m[:], diff[:], 0.0, val, op, ALU.mult),
                      reads=[r_diff], writes=[r_ident])

            CTl = sb("CTl", [128, 2, 8], F32)
            SCf = sb("SCf", [128, 2, 8], F32)
            SC = sb("SC", [128, 2, 8, 128], F32)
            BADA = sb("BADA", [128, 6 * D], F32)
            MODC = sb("MODC", [128, 2 * D], F32)
            r_ct, r_sc, r_bada, r_modc = Res(), Res(), Res(), Res()
            for j, h in enumerate((c_h, cctx_h)):
                P.dma(lambda e, j=j, h=h: e.dma_start(out=CTl[:, j, :], in_=sap(h, 0, [[1, 128], [128, 8]]),
                                                     allow_slow_non_contiguous=True), pwrites=[r_ct])
            P.dma(lambda e: e.dma_start(out=BADA[:], in_=sap(bada_h, 0, [[0, 128], [1, 6 * D]])), writes=[r_bada])
            P.act(lambda e: e.activation(SCf[:], CTl[:], AF.Silu), reads=[r_ct], writes=[r_sc])
            P.dve(lambda e: e.tensor_copy(sap(SC, 0, [[2048, 128], [128, 16], [1, 128]]),
                                          sap(SCf, 0, [[16, 128], [1, 16], [0, 128]])), reads=[r_sc], writes=[r_sc])
            WA = Rot([(sb("WA%d" % i, [128, 8, 512], F32), Res()) for i in range(3)])
            wada_v = wada_h.ap().rearrange("(k p) n -> p k n", p=128)
            pb = Rot([bank(0), bank(1), bank(2), bank(3)])
            for g in range(12):
                wa, rwa = WA.next()
                P.dma(lambda e, wa=wa, g=g: e.dma_start(out=wa[:], in_=wada_v[:, :, g * 512:(g + 1) * 512]), writes=[rwa])
                for j in range(2):
                    if j == 1 and g >= 4:
                        continue
                    pt, po, pr = pb.next()
                    for k in range(8):
                        P.pe(lambda e, pt=pt, po=po, wa=wa, j=j, k=k: e.matmul(
                            pt[:, po:po + 512], SC[:, j, k, :], wa[:, k, :], start=(k == 0), stop=(k == 7)),
                            reads=[rwa, r_sc], writes=[pr] if k == 0 else (), pwrites=() if k == 0 else [pr])
                    dst = MODL if j == 0 else MODC
                    rd = r_MODL if j == 0 else r_modc
                    P.dve(lambda e, dst=dst, pt=pt, po=po, g=g: e.tensor_tensor(
                        dst[:, g * 512:(g + 1) * 512], pt[:, po:po + 512], BADA[:, g * 512:(g + 1) * 512], ALU.add),
                        reads=[pr, r_bada], pwrites=[rd])
            P.dve(lambda e: e.tensor_scalar_add(MODL[:, 1024:2048], MODL[:, 1024:2048], 1.0), reads=[r_MODL], writes=[r_MODL])
            P.dve(lambda e: e.tensor_scalar_add(MODL[:, 4096:5120], MODL[:, 4096:5120], 1.0), reads=[r_MODL], writes=[r_MODL])
            P.dve(lambda e: e.tensor_scalar_add(MODC[:, 1024:2048], MODC[:, 1024:2048], 1.0), reads=[r_modc], writes=[r_modc])
            for idx in range(32):
                src = MODL if idx < 16 else MODC
                rs = r_MODL if idx < 16 else r_modc
                col0 = (idx % 16) * 128
                pt, po, pr = pb.next()
                P.pe(lambda e, pt=pt, po=po, src=src, col0=col0: e.transpose(pt[:, po:po + 128], src[:, col0:col0 + 128], ident_f[:]),
                     reads=[rs, r_ident], writes=[pr])
                eng = P.dve if idx % 2 == 0 else P.act
                if idx % 2 == 0:
                    P.dve(lambda e, pt=pt, po=po, idx=idx: e.tensor_copy(FM[:, idx:idx + 1], pt[:, po:po + 1]), reads=[pr], pwrites=[r_FM])
                else:
                    P.act(lambda e, pt=pt, po=po, idx=idx: e.copy(FM[:, idx:idx + 1], pt[:, po:po + 1]), reads=[pr], pwrites=[r_FM])
            if debug:
                dbg_mod = nc.dram_tensor("dbg_mod", [128, 6 * D], F32, kind="ExternalOutput")
                dbg_fm = nc.dram_tensor("dbg_fm", [128, 32], F32, kind="ExternalOutput")
                P.dma(lambda e: e.dma_start(out=dbg_mod.ap(), in_=MODL[:]), reads=[r_MODL])
                P.dma(lambda e: e.dma_start(out=dbg_fm.ap(), in_=FM[:]), reads=[r_FM])
            P.barrier()

        if upto >= 1:
            with ExitStack() as st:
                def sb(name, shape, dt):
                    return st.enter_context(nc.sbuf_tensor(name, list(shape), dt))

                TB = sb("TB", [128, 2, S], BF16)
                r_tb = Res()
                with ExitStack() as st2:
                    pi = st2.enter_context(nc.sbuf_tensor("rp_pi", [96, 1], I32))
                    pf = st2.enter_context(nc.sbuf_tensor("rp_pf", [96, 4], F32))
                    ti = st2.enter_context(nc.sbuf_tensor("rp_ti", [96, 2, S], I32))
                    tf = st2.enter_context(nc.sbuf_tensor("rp_tf", [96, 2, S], F32))
                    tf2 = st2.enter_context(nc.sbuf_tensor("rp_tf2", [96, 2, S], F32))
                    pi2 = st2.enter_context(nc.sbuf_tensor("rp_pi2", [96, 1], I32))
                    r_rp = Res()
                    P.pool(lambda e: e.iota(pi[:], pattern=[[0, 1]], base=0, channel_multiplier=1), writes=[r_rp])
                    P.pool(lambda e: e.iota(ti[:, 0, :], pattern=[[1, 64], [0, 64]], base=0, channel_multiplier=0), pwrites=[r_rp])
                    P.pool(lambda e: e.iota(ti[:, 1, :], pattern=[[0, 64], [1, 64]], base=0, channel_multiplier=0), pwrites=[r_rp])
                    P.dve(lambda e: e.tensor_copy(pf[:, 0:1], pi[:]), reads=[r_rp], writes=[r_rp])
                    P.dve(lambda e: e.tensor_copy(tf[:], ti[:]), reads=[r_rp], writes=[r_rp])
                    P.dve(lambda e: e.tensor_single_scalar(pi2[:], pi[:], 7, ALU.bitwise_and), reads=[r_rp], writes=[r_rp])
                    P.dve(lambda e: e.tensor_copy(pf[:, 1:2], pi2[:]), reads=[r_rp], writes=[r_rp])
                    P.dve(lambda e: e.tensor_single_scalar(pi2[:], pi[:], 16, ALU.bitwise_and), reads=[r_rp], writes=[r_rp])
                    P.dve(lambda e: e.tensor_copy(pf[:, 2:3], pi2[:]), reads=[r_rp], writes=[r_rp])
                    P.dve(lambda e: e.tensor_scalar_mul(pf[:, 2:3], pf[:, 2:3], 1.0 / 16), reads=[r_rp], writes=[r_rp])
                    P.act(lambda e: e.activation(pf[:, 3:4], pf[:, 1:2], AF.Exp, scale=-math.log(10000.0) / 8.0), reads=[r_rp], writes=[r_rp])
                    P.dve(lambda e: e.tensor_tensor(tf[:, 1, :], tf[:, 1, :], tf[:, 0, :], ALU.subtract), reads=[r_rp], writes=[r_rp])
                    P.dve(lambda e: e.scalar_tensor_tensor(tf[:, 0, :], tf[:, 1, :], pf[:, 2:3], tf[:, 0, :], ALU.mult, ALU.add), reads=[r_rp], writes=[r_rp])
                    P.dve(lambda e: e.tensor_scalar_mul(tf[:, 0, :], tf[:, 0, :], pf[:, 3:4]), reads=[r_rp], writes=[r_rp])
                    P.dve(lambda e: e.tensor_scalar(tf[:, 1, :], tf[:, 0, :], 0.5 / math.pi, 0.25, ALU.mult, ALU.add), reads=[r_rp], writes=[r_rp])
                    P.dve(lambda e: e.tensor_scalar_mul(tf[:, 0, :], tf[:, 0, :], 0.5 / math.pi), reads=[r_rp], writes=[r_rp])
                    P.dve(lambda e: e.tensor_copy(ti[:], tf[:]), reads=[r_rp], writes=[r_rp])
                    P.dve(lambda e: e.tensor_copy(tf2[:], ti[:]), reads=[r_rp], writes=[r_rp])
                    P.dve(lambda e: e.tensor_tensor(tf[:], tf[:], tf2[:], ALU.subtract), reads=[r_rp], writes=[r_rp])
                    P.dve(lambda e: e.tensor_scalar_mul(tf[:], tf[:], 2 * math.pi), reads=[r_rp], writes=[r_rp])
                    P.dve(lambda e: e.tensor_scalar(tf2[:], tf[:], math.pi, -2 * math.pi, ALU.is_gt, ALU.mult), reads=[r_rp], writes=[r_rp])
                    P.dve(lambda e: e.tensor_tensor(tf[:], tf[:], tf2[:], ALU.add), reads=[r_rp], writes=[r_rp])
                    P.act(lambda e: e.activation(TB[0:96, 0, :], tf[:, 1, :], AF.Sin), reads=[r_rp], pwrites=[r_tb])
                    P.act(lambda e: e.activation(TB[0:96, 1, :], tf[:, 0, :], AF.Sin), reads=[r_rp], pwrites=[r_tb])
                    P.dve(lambda e: e.memset(TB[0:64, 0, :], 1.0), reads=[r_tb], writes=[r_tb])
                    P.dve(lambda e: e.memset(TB[0:64, 1, :], 0.0), reads=[r_tb], writes=[r_tb])
                    P.barrier()

                WIN = sb("WIN", [128, 8, 1984], BF16)
                WKRB = sb("WKRB", [128, 8, 96], BF16)
                WKRA = sb("WKRA", [128, 8, 96], BF16)
                WUQA = sb("WUQA", [128, 2, 768], BF16)
                WUQB = sb("WUQB", [128, 2, 768], BF16)
                QG = sb("QG", [128, 2], F32)
                KVG = sb("KVG", [128, 1], F32)
                WUKVK = sb("WUKVK", [128, 4, 128], BF16)
                WUKVV = sb("WUKVV", [128, 512], BF16)
                WGK = sb("WGK", [64, 512], BF16)
                XB = [sb("XB%d" % i, [128, 4, D], F32) for i in range(2)]
                UTs = [sb("UT%d" % i, [128, 8, 512], BF16) for i in range(2)]
                GTA = [sb("GTA%d" % i, [64, 512], BF16) for i in range(2)]
                CQT = sb("CQT", [128, 3, 512], BF16)
                SQ = sb("SQ", [128, 3, 512], F32)
                RQ = sb("RQ", [128, 512], F32)
                RKV = sb("RKV", [128, 512], F32)
                RKVT = sb("RKVT", [128, 4], F32)
                TBr = sb("TBr", [128, 2, 512], F32)
                STG = Rot([(sb("STG%d" % i, [128, 512], BF16), Res()) for i in range(2)])
                TMP = Rot([(sb("TMP%d" % i, [128, 512], F32), Res()) for i in range(4)])
                SPst = sb("SPst", [128, 4, 512], BF16)
                VDst = sb("VDst", [128, 4, 512], BF16)
                GKst = sb("GKst", [128, 4, 256], BF16)
                GVst = sb("GVst", [128, 4, 512], BF16)
                GRst = sb("GRst", [128, 4, 512], BF16)
                GQst = sb("GQst", [128, 2, 512], BF16)
                GK2st = sb("GK2st", [128, 2, 512], BF16)
                QTst = sb("QTst", [128, 8, 512], BF16)
                KNst = sb("KNst", [128, 4, 512], BF16)
                r_spst, r_vdst, r_gkst, r_gvst, r_grst, r_gqst, r_gk2st, r_qtst, r_knst = [Res() for _ in range(9)]
                r_w = Res()
                r_xb = [Res(), Res()]
                r_ut = [Res(), Res()]
                r_gta = [Res(), Res()]
                r_cqt, r_sq, r_rq, r_rkv, r_rkvt, r_tbr = Res(), Res(), Res(), Res(), Res(), Res()
                r_pt = [Res(), Res()]
                pb = Rot([bank(4), bank(5), bank(6), bank(7)])

                win_v = win_h.ap().rearrange("(k p) n -> p k n", p=128)
                WST = Rot([(XB[i], r_xb[i]) for i in range(2)])
                for k in range(8):
                    ws, rws = WST.next()
                    P.dma(lambda e, k=k, ws=ws: e.dma_start(out=sap(ws, 0, [[4096, 128], [1, 1984]]), in_=win_v[:, k, :]), writes=[rws])
                    if k % 2 == 0:
                        P.dve(lambda e, k=k, ws=ws: e.tensor_copy(WIN[:, k, :], sap(ws, 0, [[4096, 128], [1, 1984]])), reads=[rws], pwrites=[r_w])
                    else:
                        P.act(lambda e, k=k, ws=ws: e.copy(WIN[:, k, :], sap(ws, 0, [[4096, 128], [1, 1984]])), reads=[rws], pwrites=[r_w])
                ws, rws = WST.next()
                P.dma(lambda e, ws=ws: e.dma_start(out=sap(ws, 0, [[4096, 33], [1, 512]]), in_=wgk_h.ap()), writes=[rws])
                r_wgk = Res()
                P.dve(lambda e: e.memset(WGK[:], 0.0), writes=[r_wgk])
                P.dve(lambda e, ws=ws: e.tensor_copy(WGK[0:33, :], sap(ws, 0, [[4096, 33], [1, 512]])), reads=[rws, r_wgk], writes=[r_wgk])
                WUQf = sap(XB[0], 0, [[4096, 128], [768, 2], [1, 768]])
                WUKVf_h = XB[1]
                r_wq, r_wkv = r_xb[0], r_xb[1]
                P.dma(lambda e: e.dma_start(out=WUQf, in_=wuq_h.ap().rearrange("(k p) n -> p k n", p=128)), writes=[r_wq])
                P.dma(lambda e: e.dma_start(out=sap(WUKVf_h, 0, [[4096, 128], [1, 1024]]), in_=wukv_h.ap()), writes=[r_wkv])
                P.dma(lambda e: e.dma_start(out=QG[:], in_=sap(qg_h, 0, [[1, 128], [128, 2]]), allow_slow_non_contiguous=True), pwrites=[r_w])
                P.dma(lambda e: e.dma_start(out=KVG[:], in_=sap(kvg_h, 0, [[1, 128], [1, 1]])), pwrites=[r_w])
                r_w2 = Res()
                P.dve(lambda e: e.memset(WKRA[:], 0.0), writes=[r_w2])
                P.dve(lambda e: e.memset(WKRB[:], 0.0), reads=[r_w2], writes=[r_w2])
                P.dve(lambda e: e.tensor_copy(WKRA[:, :, 64:96], WIN[:, :, 1952:1984]), reads=[r_w, r_w2], writes=[r_w2])
                for a in range(2):
                    c0 = 1952 + a * 16
                    P.dve(lambda e, a=a, c0=c0: e.tensor_scalar_mul(WKRB[:, :, 64 + a * 16:64 + a * 16 + 8], WIN[:, :, c0 + 8:c0 + 16], -1.0), reads=[r_w, r_w2], writes=[r_w2])
                    P.dve(lambda e, a=a, c0=c0: e.tensor_copy(WKRB[:, :, 64 + a * 16 + 8:64 + a * 16 + 16], WIN[:, :, c0:c0 + 8]), reads=[r_w, r_w2], writes=[r_w2])
                P.dve(lambda e: e.memset(WUQB[:], 0.0), reads=[r_w2], writes=[r_w2])
                for kc in range(2):
                    P.dve(lambda e, kc=kc: e.tensor_scalar_mul(WUQA[:, kc, :], sap(XB[0], kc * 768, [[4096, 128], [1, 768]]), QG[:, kc:kc + 1]), reads=[r_w, r_wq], pwrites=[r_w2])
                for kc in range(2):
                    A3 = lambda off, kc=kc: sap(WUQA, kc * 768 + off, [[1536, 128], [96, 8], [1, 8]])
                    B3 = lambda off, kc=kc: sap(WUQB, kc * 768 + off, [[1536, 128], [96, 8], [1, 8]])
                    for a in range(2):
                        o = 64 + a * 16
                        P.dve(lambda e, o=o, A3=A3, B3=B3: e.tensor_scalar_mul(B3(o), A3(o + 8), -1.0), reads=[r_w2], pwrites=[r_w2])
                        P.dve(lambda e, o=o, A3=A3, B3=B3: e.tensor_copy(B3(o + 8), A3(o)), reads=[r_w2], pwrites=[r_w2])
                P.dve(lambda e: e.tensor_scalar_mul(sap(WUKVK, 0, [[512, 128], [64, 8], [1, 64]]),
                                                    sap(WUKVf_h, 0, [[4096, 128], [128, 8], [1, 64]]), KVG[:, 0:1]), reads=[r_w, r_wkv], pwrites=[r_w2])
                P.dve(lambda e: e.tensor_scalar_mul(sap(WUKVV, 0, [[512, 128], [64, 8], [1, 64]]),
                                                    sap(WUKVf_h, 64, [[4096, 128], [128, 8], [1, 64]]), KVG[:, 0:1]), reads=[r_w, r_wkv], pwrites=[r_w2])
                for i in range(2):
                    P.dve(lambda e, i=i: e.memset(GTA[i][:], 0.0), writes=[r_gta[i]])
                    P.dve(lambda e, i=i: e.memset(GTA[i][32:33, :], 1.0), writes=[r_gta[i]])

                blocks = [(0, 2, True)] + [(CT + 512 * b, 4, False) for b in range(8)]

                def src_rows(tok0, t):
                    if tok0 < CT:
                        return ctx_h.ap()[tok0 + t * 128: tok0 + (t + 1) * 128, :]
                    s0 = tok0 - CT + t * 128
                    return x_h.ap()[s0:s0 + 128, :]

                def load_block(bi):
                    tok0, ntl, _ = blocks[bi]
                    xb = XB[bi % 2]
                    if tok0 < CT:
                        src = ctx_h.ap().rearrange("(t p) d -> p t d", p=128)
                    else:
                        src = x_h.ap()[tok0 - CT:tok0 - CT + ntl * 128, :].rearrange("(t p) d -> p t d", p=128)
                    P.dma(lambda e, xb=xb, src=src, ntl=ntl: e.dma_start(out=xb[:, 0:ntl, :], in_=src), writes=[r_xb[bi % 2]])

                def mm_acc(pt, po, pr, M, N, lhs, rhs, nk, reads, f32=False):
                    for k in range(nk):
                        P.pe(lambda e, k=k: e.matmul(pt[0:M, po:po + N], lhs(k), rhs(k), start=(k == 0), stop=(k == nk - 1)),
                             reads=reads, writes=[pr] if k == 0 else (), pwrites=() if k == 0 else [pr])

                def do_block(bi, tok0, ntl, is_ctx):
                    if bi + 1 < len(blocks):
                        load_block(bi + 1)
                    NB = ntl * 128
                    s0 = tok0 - CT
                    xb, rxb = XB[bi % 2], r_xb[bi % 2]
                    UT, rut = UTs[bi % 2], r_ut[bi % 2]
                    gta, rgta = GTA[bi % 2], r_gta[bi % 2]
                    fsh, fsc = (16, 24) if is_ctx else (0, 8)
                    for t in range(ntl):
                        PT, rpt = PSt[t % 2], r_pt[t % 2]
                        for k in range(8):
                            P.pe(lambda e, PT=PT, xb=xb, t=t, k=k: e.transpose(PT[:, k * 128:(k + 1) * 128], xb[:, t, k * 128:(k + 1) * 128], ident_f[:]),
                                 reads=[rxb, r_ident], writes=[rpt] if k == 0 else (), pwrites=() if k == 0 else [rpt])
                        for k in range(8):
                            wr = dict(writes=[rut]) if (t == 0 and k == 0) else dict(pwrites=[rut])
                            if True:
                                P.dve(lambda e, PT=PT, UT=UT, t=t, k=k: e.tensor_scalar(
                                    UT[:, k, t * 128:(t + 1) * 128], PT[:, k * 128:(k + 1) * 128],
                                    FM[:, fsc + k:fsc + k + 1], FM[:, fsh + k:fsh + k + 1], ALU.mult, ALU.add), reads=[rpt, r_FM], **wr)
                            else:
                                P.act(lambda e, PT=PT, UT=UT, t=t, k=k: e.activation(
                                    UT[:, k, t * 128:(t + 1) * 128], PT[:, k * 128:(k + 1) * 128], AF.Identity,
                                    bias=FM[:, fsh + k:fsh + k + 1], scale=FM[:, fsc + k:fsc + k + 1]), reads=[rpt, r_FM], **wr)
                    if DEV_STAGE <= 1:
                        return
                    rw = [r_w, r_w2, rut]
                    U = lambda k: UT[:, k, 0:NB]
                    for c in range(4):
                        pt, po, pr = pb.next()
                        mm_acc(pt, po, pr, 128, NB, lambda k, c=c: WIN[:, k, c * 128:(c + 1) * 128], U, 8, rw)
                        cc = c % 2
                        wr = (lambda r: dict(writes=[r]) if cc == 0 else dict(pwrites=[r]))
                        if c < 2:
                            P.act(lambda e, pt=pt, po=po, cc=cc: e.activation(GQst[:, cc, 0:NB], pt[:, po:po + NB], AF.Copy, scale=0.125), reads=[pr], **wr(r_gqst))
                        else:
                            P.dve(lambda e, pt=pt, po=po, cc=cc: e.tensor_copy(GK2st[:, cc, 0:NB], pt[:, po:po + NB]), reads=[pr], **wr(r_gk2st))
                        if cc == 1:
                            stt, rst, dst, rdst = (GQst, r_gqst, GQT, R_["GQT"]) if c < 2 else (GK2st, r_gk2st, GKT, R_["GKT"])
                            P.dma(lambda e, stt=stt, dst=dst: e.dma_start(out=dst.ap().rearrange("(c p) t -> p c t", p=128)[:, :, tok0:tok0 + NB], in_=stt[:, :, 0:NB]),
                                  reads=[rst], pwrites=[rdst])
                    if DEV_STAGE <= 2:
                        return
                    pt, po, pr = pb.next()
                    mm_acc(pt, po, pr, 32, NB, lambda k: WIN[:, k, 1024:1056], U, 8, rw)
                    P.act(lambda e, pt=pt, po=po, gta=gta: e.copy(gta[0:32, 0:NB], pt[0:32, po:po + NB]), reads=[pr], writes=[rgta])
                    for t in range(ntl):
                        pt, po, pr = pb.next()
                        P.pe(lambda e, pt=pt, po=po, gta=gta, t=t: e.matmul(pt[:, po:po + 512], gta[0:64, t * 128:(t + 1) * 128], WGK[0:64, :], start=True, stop=True),
                             reads=[rgta, r_wgk], writes=[pr])
                        tm, rtm = TMP.next()
                        P.act(lambda e, tm=tm, pt=pt, po=po: e.activation(tm[:], pt[:, po:po + 512], AF.Exp, scale=-1.0), reads=[pr], writes=[rtm])
                        P.act(lambda e, tm=tm, t=t: e.activation(SPst[:, t, :], tm[:], AF.Ln, bias=1.0), reads=[rtm],
                              writes=[r_spst] if t == 0 else (), pwrites=() if t == 0 else [r_spst])
                    P.dma(lambda e: e.dma_start(out=SPD.ap()[tok0:tok0 + NB, :].rearrange("(t p) c -> p t c", p=128), in_=SPst[:, 0:ntl, :]),
                          reads=[r_spst], pwrites=[R_["SPD"]])
                    if DEV_STAGE <= 3:
                        return
                    for c in range(3):
                        if c < 2 and is_ctx:
                            continue
                        pt, po, pr = pb.next()
                        c0 = 1568 + c * 128
                        mm_acc(pt, po, pr, 128, NB, lambda k, c0=c0: WIN[:, k, c0:c0 + 128], U, 8, rw)
                        P.dve(lambda e, pt=pt, po=po, c=c: e.tensor_copy(CQT[:, c, 0:NB], pt[:, po:po + NB]), reads=[pr], pwrites=[r_cqt])
                        P.act(lambda e, pt=pt, po=po, c=c: e.activation(SQ[:, c, 0:NB], CQT[:, c, 0:NB], AF.Square), reads=[r_cqt], pwrites=[r_sq])
                    if (not is_ctx) and DEV_RQ:
                        pt, po, pr = pb.next()
                        mm_acc(pt, po, pr, 128, NB, lambda k: ones_f[:], lambda k: SQ[:, k, 0:NB], 2, [r_sq, r_ident])
                        P.dve(lambda e, pt=pt, po=po: e.tensor_scalar(RQ[:, 0:NB], pt[:, po:po + NB], 96.0 / 256, 96.0 * EPS, ALU.mult, ALU.add), reads=[pr], writes=[r_rq])
                        P.act(lambda e: e.activation(RQ[:, 0:NB], RQ[:, 0:NB], AF.Ln), reads=[r_rq], writes=[r_rq])
                        P.act(lambda e: e.activation(RQ[:, 0:NB], RQ[:, 0:NB], AF.Exp, scale=-0.5), reads=[r_rq], writes=[r_rq])
                    pt, po, pr = pb.next()
                    mm_acc(pt, po, pr, 128, NB, lambda k: ones_f[:], lambda k: SQ[:, 2, 0:NB], 1, [r_sq, r_ident])
                    P.dve(lambda e, pt=pt, po=po: e.tensor_scalar(RKV[:, 0:NB], pt[:, po:po + NB], 1.0 / 128, EPS, ALU.mult, ALU.add), reads=[pr], writes=[r_rkv])
                    P.act(lambda e: e.activation(RKV[:, 0:NB], RKV[:, 0:NB], AF.Ln), reads=[r_rkv], writes=[r_rkv])
                    P.act(lambda e: e.activation(RKV[:, 0:NB], RKV[:, 0:NB], AF.Exp, scale=-0.5), reads=[r_rkv], writes=[r_rkv])
                    pt, po, pr = pb.next()
                    for t in range(ntl):
                        P.pe(lambda e, pt=pt, po=po, t=t: e.matmul(pt[:, po + t * 128:po + (t + 1) * 128], SQ[:, 2, t * 128:(t + 1) * 128], ones_f[:], start=True, stop=True),
                             reads=[r_sq, r_ident], writes=[pr] if t == 0 else (), pwrites=() if t == 0 else [pr])
                    P.dve(lambda e, pt=pt, po=po: e.tensor_scalar(RKVT[:, 0:ntl], sap(pt, po, [[1024, 128], [128, ntl]]), 1.0 / 128, EPS, ALU.mult, ALU.add), reads=[pr], writes=[r_rkvt])
                    P.act(lambda e: e.activation(RKVT[:, 0:ntl], RKVT[:, 0:ntl], AF.Ln), reads=[r_rkvt], writes=[r_rkvt])
                    P.act(lambda e: e.activation(RKVT[:, 0:ntl], RKVT[:, 0:ntl], AF.Exp, scale=-0.5), reads=[r_rkvt], writes=[r_rkvt])
                    if DEV_STAGE <= 4:
                        return
                    if not is_ctx:
                        for j in range(2):
                            P.dve(lambda e, j=j: e.tensor_tensor(TBr[0:96, j, :], TB[0:96, j, s0:s0 + 512], RQ[0:96, :], ALU.mult),
                                  reads=[r_tb, r_rq], writes=[r_tbr] if j == 0 else (), pwrites=() if j == 0 else [r_tbr])
                        for h in range(8):
                            pa, poa, pra = pb.next()
                            mm_acc(pa, poa, pra, 96, 512, lambda k, h=h: WUQA[:, k, h * 96:(h + 1) * 96], lambda k: CQT[:, k, :], 2, [r_w2, r_cqt])
                            pb_, pob, prb = pb.next()
                            mm_acc(pb_, pob, prb, 96, 512, lambda k, h=h: WUQB[:, k, h * 96:(h + 1) * 96], lambda k: CQT[:, k, :], 2, [r_w2, r_cqt])
                            t1, rt1 = TMP.next()
                            t2, rt2 = TMP.next()
                            P.dve(lambda e, t1=t1, pa=pa, poa=poa: e.tensor_tensor(t1[0:96, :], pa[0:96, poa:poa + 512], TBr[0:96, 0, :], ALU.mult), reads=[pra, r_tbr], writes=[rt1])
                            P.dve(lambda e, t2=t2, pb_=pb_, pob=pob: e.tensor_tensor(t2[0:96, :], pb_[0:96, pob:pob + 512], TBr[0:96, 1, :], ALU.mult), reads=[prb, r_tbr], writes=[rt2])
                            P.dve(lambda e, h=h, t1=t1, t2=t2: e.tensor_tensor(QTst[0:96, h, :], t1[0:96, :], t2[0:96, :], ALU.add), reads=[rt1, rt2],
                                  writes=[r_qtst] if h == 0 else (), pwrites=() if h == 0 else [r_qtst])
                        P.dma(lambda e: e.dma_start(out=QTD.ap().rearrange("h r t -> r h t")[:, :, s0:s0 + 512], in_=QTst[0:96, :, :]), reads=[r_qtst], pwrites=[R_["QTD"]])
                    for pr_i in range(4):
                        pt, po, pr = pb.next()
                        mm_acc(pt, po, pr, 128, NB, lambda k, pr_i=pr_i: WUKVK[:, pr_i, :], lambda k: CQT[:, 2, 0:NB], 1, [r_w2, r_cqt])
                        P.dve(lambda e, pr_i=pr_i, pt=pt, po=po: e.tensor_tensor(KNst[:, pr_i, 0:NB], pt[:, po:po + NB], RKV[:, 0:NB], ALU.mult), reads=[pr, r_rkv],
                              writes=[r_knst] if pr_i == 0 else (), pwrites=() if pr_i == 0 else [r_knst])
                    P.dma(lambda e: e.dma_start(out=KNT.ap().rearrange("q r t -> r q t")[:, :, tok0:tok0 + NB], in_=KNst[:, :, 0:NB]), reads=[r_knst], pwrites=[R_["KNT"]])
                    if DEV_STAGE <= 5:
                        return
                    pa, poa, pra = pb.next()
                    mm_acc(pa, poa, pra, 96, NB, lambda k: WKRA[:, k, :], U, 8, rw)
                    sg, rsg = STG.next()
                    if is_ctx:
                        P.act(lambda e, sg=sg, pa=pa, poa=poa: e.copy(sg[0:96, 0:NB], pa[0:96, poa:poa + NB]), reads=[pra], writes=[rsg])
                    else:
                        pb_, pob, prb = pb.next()
                        mm_acc(pb_, pob, prb, 96, NB, lambda k: WKRB[:, k, :], U, 8, rw)
                        t1, rt1 = TMP.next()
                        t2, rt2 = TMP.next()
                        P.dve(lambda e, t1=t1, pa=pa, poa=poa: e.tensor_tensor(t1[0:96, :], pa[0:96, poa:poa + 512], TB[0:96, 0, s0:s0 + 512], ALU.mult), reads=[pra, r_tb], writes=[rt1])
                        P.dve(lambda e, t2=t2, pb_=pb_, pob=pob: e.tensor_tensor(t2[0:96, :], pb_[0:96, pob:pob + 512], TB[0:96, 1, s0:s0 + 512], ALU.mult), reads=[prb, r_tb], writes=[rt2])
                        P.dve(lambda e, sg=sg, t1=t1, t2=t2: e.tensor_tensor(sg[0:96, :], t1[0:96, :], t2[0:96, :], ALU.add), reads=[rt1, rt2], writes=[rsg])
                    P.dma(lambda e, sg=sg: e.dma_start(out=KRT.ap()[:, tok0:tok0 + NB], in_=sg[64:96, 0:NB]), reads=[rsg], pwrites=[R_["KRT"]])
                    if DEV_STAGE <= 6:
                        return
                    for t in range(ntl):
                        lhs = lambda k, t=t: UT[:, k, t * 128:(t + 1) * 128]
                        wrs = (lambda r: dict(writes=[r]) if t == 0 else dict(pwrites=[r]))
                        pt, po, pr = pb.next()
                        P.pe(lambda e, pt=pt, po=po, t=t: e.matmul(pt[:, po:po + 512], CQT[:, 2, t * 128:(t + 1) * 128], WUKVV[:], start=True, stop=True),
                             reads=[r_cqt, r_w2], writes=[pr])
                        P.dve(lambda e, pt=pt, po=po, t=t: e.tensor_scalar_mul(VDst[:, t, :], pt[:, po:po + 512], RKVT[:, t:t + 1]), reads=[pr, r_rkvt], **wrs(r_vdst))
                        pt, po, pr = pb.next()
                        mm_acc(pt, po, pr, 128, 256, lhs, lambda k: WIN[:, k, 256:512], 8, rw)
                        P.dve(lambda e, pt=pt, po=po, t=t: e.tensor_copy(GKst[:, t, :], pt[:, po:po + 256]), reads=[pr], **wrs(r_gkst))
                        pt, po, pr = pb.next()
                        mm_acc(pt, po, pr, 128, 512, lhs, lambda k: WIN[:, k, 512:1024], 8, rw)
                        P.act(lambda e, pt=pt, po=po, t=t: e.copy(GVst[:, t, :], pt[:, po:po + 512]), reads=[pr], **wrs(r_gvst))
                        if not is_ctx:
                            pt, po, pr = pb.next()
                            mm_acc(pt, po, pr, 128, 512, lhs, lambda k: WIN[:, k, 1056:1568], 8, rw)
                            P.act(lambda e, pt=pt, po=po, t=t: e.activation(GRst[:, t, :], pt[:, po:po + 512], AF.Silu), reads=[pr], **wrs(r_grst))
                    rows = lambda dram, r0: dram.ap()[r0:r0 + NB, :].rearrange("(t p) c -> p t c", p=128)
                    P.dma(lambda e: e.dma_start(out=rows(VD, tok0), in_=VDst[:, 0:ntl, :]), reads=[r_vdst], pwrites=[R_["VD"]])
                    P.dma(lambda e: e.dma_start(out=rows(GKD, tok0), in_=GKst[:, 0:ntl, :]), reads=[r_gkst], pwrites=[R_["GKD"]])
                    P.dma(lambda e: e.dma_start(out=rows(GVD, tok0), in_=GVst[:, 0:ntl, :]), reads=[r_gvst], pwrites=[R_["GVD"]])
                    if not is_ctx:
                        P.dma(lambda e: e.dma_start(out=rows(GRD, s0), in_=GRst[:, 0:ntl, :]), reads=[r_grst], pwrites=[R_["GRD"]])

                load_block(0)
                for bi, blk in enumerate(blocks[:DEV_NBLK]):
                    do_block(bi, *blk)
                P.barrier()

        def drive(gs):
            gs = list(gs)
            while gs:
                for g in list(gs):
                    try:
                        next(g)
                    except StopIteration:
                        gs.remove(g)

        if upto >= 2:
            P.default_q = "pool"
            OBD = scratch("OBD", [S, 512], F32)
            R_["OBD"] = Res("OBD")
            with ExitStack() as st:
                def sb(name, shape, dt):
                    return st.enter_context(nc.sbuf_tensor(name, list(shape), dt))

                QT2 = sb("QT2", [128, 2, T], BF16)
                KT2 = sb("KT2", [128, 2, T], BF16)
                KTM = sb("KTM", [128, NT, 256], BF16)
                VTM = sb("VTM", [128, NT, 512], BF16)
                SPT = sb("SPT", [128, NT, 512], BF16)
                r_in = Res()
                NBUF = 2
                GS_ = [sb("GS_%d" % d, [128, 256], F32) for d in range(2)]
                GSbl = [sb("GSbl%d" % d, [128, 256], BF16) for d in range(2)]
                GSbh = [sb("GSbh%d" % d, [128, 256], BF16) for d in range(2)]
                r_S = [Res(), Res()]
                r_Sb = [Res(), Res()]
                mkb = lambda nm, shp, dt: [[sb("%s%d_%d" % (nm, d, i), shp, dt) for i in range(NBUF)] for d in range(2)]
                gEQ = mkb("EQ", [128, 256], F32); gEK = mkb("EK", [128, 256], F32); gEKD = mkb("EKD", [128, 256], F32)
                gQE = mkb("QE", [128, 256], BF16); gKEl = mkb("KEl", [128, 256], BF16); gKEh = mkb("KEh", [128, 256], BF16)
                gKD = mkb("KD", [128, 256], BF16); gAT = mkb("AT", [128, 512], BF16)
                gr_e = [[Res() for _ in range(NBUF)] for _ in range(2)]
                gr_qk = [[Res() for _ in range(NBUF)] for _ in range(2)]
                gr_at = [[Res() for _ in range(NBUF)] for _ in range(2)]
                gOST = [sb("OST%d" % d, [128, 4, 512], F32) for d in range(2)]
                gr_ost = [Res(), Res()]

                for p2 in range(2):
                    P.dma(lambda e, p2=p2: e.dma_start(out=QT2[:, p2, :], in_=GQT.ap()[p2 * 128:(p2 + 1) * 128, :]), reads=[R_["GQT"]], pwrites=[r_in])
                    P.dma(lambda e, p2=p2: e.dma_start(out=KT2[:, p2, :], in_=GKT.ap()[p2 * 128:(p2 + 1) * 128, :]), reads=[R_["GKT"]], pwrites=[r_in])
                P.dma(lambda e: e.dma_start(out=KTM[:], in_=GKD.ap().rearrange("(n p) c -> p n c", p=128)), reads=[R_["GKD"]], pwrites=[r_in])
                P.dma(lambda e: e.dma_start(out=VTM[:], in_=GVD.ap().rearrange("(n p) c -> p n c", p=128)), reads=[R_["GVD"]], pwrites=[r_in])
                P.dma(lambda e: e.dma_start(out=SPT[:], in_=SPD.ap().rearrange("(n p) c -> p n c", p=128)), reads=[R_["SPD"]], pwrites=[r_in])
                for d in range(2):
                    for i in range(NBUF):
                        P.dve(lambda e, d=d, i=i: e.memset(gKEl[d][i][:], 0.0), writes=[gr_qk[d][i]])
                        P.dve(lambda e, d=d, i=i: e.memset(gKEh[d][i][:], 0.0), pwrites=[gr_qk[d][i]])

                def gla_dir(d):
                    cum, rem, msk = (mk["cum_f"], mk["rem_f"], mk["msk_f"]) if d == 0 else (mk["cum_b"], mk["rem_b"], mk["msk_b"])
                    lastcol = 127 if d == 0 else 0
                    order = list(range(NT)) if d == 0 else [1, 0] + list(range(NT - 1, 1, -1))
                    S_, Sbl, Sbh = GS_[d], GSbl[d], GSbh[d]
                    OD, rOD = (OFD, R_["OFD"]) if d == 0 else (OBD, R_["OBD"])
                    ost, rost = gOST[d], gr_ost[d]
                    bk = lambda j: bank(4 * d + j)
                    P.dve(lambda e: e.memset(S_[:], 0.0), writes=[r_S[d]])
                    P.dve(lambda e: e.memset(Sbl[:], 0.0), writes=[r_Sb[d]])
                    P.dve(lambda e: e.memset(Sbh[:], 0.0), pwrites=[r_Sb[d]])
                    yield

                    def pre(i):
                        tt = order[i]
                        b = i % NBUF
                        lat = tt >= 2
                        t0 = tt * 128
                        EQ, EK, EKD, QE, KEl, KEh, KD, AT = gEQ[d][b], gEK[d][b], gEKD[d][b], gQE[d][b], gKEl[d][b], gKEh[d][b], gKD[d][b], gAT[d][b]
                        r_e, r_qk, r_at = gr_e[d][b], gr_qk[d][b], gr_at[d][b]
                        pbt, pbo, pbr = bk(0)
                        for p in range(2):
                            P.pe(lambda e, p=p: e.matmul(pbt[:, pbo + p * 128:pbo + (p + 1) * 128], SPT[:, tt, d * 256 + p * 128:d * 256 + (p + 1) * 128], cum[:], start=True, stop=True),
                                 reads=[r_in, r_ident], writes=[pbr] if p == 0 else (), pwrites=() if p == 0 else [pbr])
                        P.pe(lambda e: e.matmul(pbt[:, pbo + 256:pbo + 512], rem[:], SPT[:, tt, d * 256:(d + 1) * 256], start=True, stop=True),
                             reads=[r_in, r_ident], pwrites=[pbr])
                        yield
                        P.act(lambda e: e.activation(EQ[:], pbt[:, pbo:pbo + 256], AF.Exp), reads=[pbr], writes=[r_e])
                        P.act(lambda e: e.activation(EK[:], pbt[:, pbo:pbo + 256], AF.Exp, scale=-1.0), reads=[pbr], pwrites=[r_e])
                        P.act(lambda e: e.activation(EKD[:], pbt[:, pbo + 256:pbo + 512], AF.Exp), reads=[pbr], pwrites=[r_e])
                        yield
                        v3 = lambda h_: sap(h_, 0, [[256, 128], [128, 2], [1, 128]])
                        v3h = lambda h_, r0: sap(h_, r0 * 256, [[256, 64], [128, 2], [1, 128]])
                        P.dve(lambda e: e.tensor_tensor(v3(QE), QT2[:, :, t0:t0 + 128], v3(EQ), ALU.mult), reads=[r_in, r_e], writes=[r_qk])
                        P.dve(lambda e: e.tensor_tensor(v3h(KEl, 0), KT2[0:64, :, t0:t0 + 128], v3h(EK, 0), ALU.mult), reads=[r_in, r_e], pwrites=[r_qk])
                        yield
                        P.dve(lambda e: e.tensor_tensor(v3h(KEh, 64), KT2[64:128, :, t0:t0 + 128], v3h(EK, 64), ALU.mult), reads=[r_in, r_e], pwrites=[r_qk])
                        P.dve(lambda e: e.tensor_tensor(KD[:], KTM[:, tt, :], EKD[:], ALU.mult), reads=[r_in, r_e], pwrites=[r_qk])
                        yield
                        if lat:
                            pat, pao, par = bk(1)
                            for h in range(4):
                                r0, c0 = (h % 2) * 64, (h // 2) * 128
                                P.pe(lambda e, h=h, r0=r0, c0=c0: e.matmul(pat[:, pao + h * 128:pao + (h + 1) * 128], (KEl if r0 == 0 else KEh)[:, c0:c0 + 128], QE[:, c0:c0 + 128], start=True, stop=True),
                                     reads=[r_qk], writes=[par] if h == 0 else (), pwrites=() if h == 0 else [par])
                            yield
                            P.dve(lambda e: e.tensor_tensor(sap(AT, 0, [[512, 128], [128, 4], [1, 128]]), sap(pat, pao, [[1024, 128], [128, 4], [1, 128]]),
                                                            sap(msk, 0, [[128, 128], [0, 4], [1, 128]]), ALU.mult), reads=[par, r_ident], writes=[r_at])
                            yield

                    def post(i):
                        tt = order[i]
                        b = i % NBUF
                        lat = tt >= 2
                        EQ, QE, KD, AT = gEQ[d][b], gQE[d][b], gKD[d][b], gAT[d][b]
                        r_e, r_qk, r_at = gr_e[d][b], gr_qk[d][b], gr_at[d][b]
                        pot, poo, por = bk(2)
                        pdt, pdo, pdr = bk(3)
                        if lat:
                            for h in range(4):
                                r0, c0 = (h % 2) * 64, (h // 2) * 128
                                P.pe(lambda e, h=h: e.matmul(pot[:, poo + h * 128:poo + (h + 1) * 128], AT[:, h * 128:(h + 1) * 128], VTM[:, tt, h * 128:(h + 1) * 128], start=True, stop=False),
                                     reads=[r_at, r_in], writes=[por] if h == 0 else (), pwrites=() if h == 0 else [por])
                                P.pe(lambda e, h=h, r0=r0, c0=c0: e.matmul(pot[:, poo + h * 128:poo + (h + 1) * 128], QE[:, c0:c0 + 128], (Sbl if r0 == 0 else Sbh)[:, c0:c0 + 128], start=False, stop=True),
                                     reads=[r_qk, r_Sb[d]], pwrites=[por])
                        for h in range(4):
                            r0, c0 = (h % 2) * 64, (h // 2) * 128
                            P.pe(lambda e, h=h, r0=r0, c0=c0: e.matmul(pdt[r0:r0 + 64, pdo + c0:pdo + c0 + 128], KD[:, h * 64:(h + 1) * 64], VTM[:, tt, h * 128:(h + 1) * 128], start=True, stop=True),
                                 reads=[r_qk, r_in], writes=[pdr] if h == 0 else (), pwrites=() if h == 0 else [pdr])
                        yield
                        for p in range(2):
                            col = p * 128 + lastcol
                            P.dve(lambda e, p=p, col=col: e.scalar_tensor_tensor(S_[:, p * 128:(p + 1) * 128], S_[:, p * 128:(p + 1) * 128], EQ[:, col:col + 1],
                                                                                 pdt[:, pdo + p * 128:pdo + (p + 1) * 128], ALU.mult, ALU.add),
                                  reads=[pdr, r_e, r_S[d]], writes=[r_S[d]])
                        yield
                        P.act(lambda e: e.copy(Sbl[0:64, :], S_[0:64, :]), reads=[r_S[d]], writes=[r_Sb[d]])
                        P.act(lambda e: e.copy(Sbh[64:128, :], S_[64:128, :]), reads=[r_S[d]], pwrites=[r_Sb[d]])
                        if lat:
                            lt = tt - 2
                            slot = lt % 4
                            first = (slot == 0) if d == 0 else (slot == 3)
                            last = (slot == 3) if d == 0 else (slot == 0)
                            P.act(lambda e: e.copy(ost[:, slot, :], pot[:, poo:poo + 512]), reads=[por], writes=[rost] if first else (), pwrites=() if first else [rost])
                            if last:
                                g0 = (lt // 4) * 512
                                P.dma(lambda e: e.dma_start(out=OD.ap()[g0:g0 + 512, :].rearrange("(t p) c -> p t c", p=128), in_=ost[:]), reads=[rost], pwrites=[rOD])
                        yield

                    yield from pre(0)
                    for i in range(NT):
                        gens = []
                        if i + 1 < NT:
                            gens.append(pre(i + 1))
                        gens.append(post(i))
                        while gens:
                            for g in list(gens):
                                try:
                                    next(g)
                                    yield
                                except StopIteration:
                                    gens.remove(g)

                drive([gla_dir(0), gla_dir(1)])
                P.barrier()

        if upto >= 3:
            with ExitStack() as st:
                def sb(name, shape, dt):
                    return st.enter_context(nc.sbuf_tensor(name, list(shape), dt))

                VALL = sb("VALL", [128, NT, 512], BF16)
                KTh = [sb("KTh%d" % i, [128, T], BF16) for i in range(2)]
                QTh = [sb("QTh%d" % i, [128, S], BF16) for i in range(2)]
                VA = [sb("VA%d" % i, [128, NT, 128], BF16) for i in range(2)]
                r_vall = Res()
                r_kq = [Res(), Res()]
                r_va = [Res(), Res()]
                PTs = Rot([(sb("PTs%d" % i, [128, 512], BF16), Res()) for i in range(3)])
                DEN = Rot([(sb("DEN%d" % i, [128, 512], F32), Res()) for i in range(2)])
                OTs = Rot([(sb("OTs%d" % i, [64, 512], BF16), Res()) for i in range(2)])
                psS = Rot([bank(0), bank(1), bank(2)])
                psO = Rot([bank(4), bank(5)])
                GG = sb("GG", [128, 128], F32)
                r_gg = Res()
                P.dma(lambda e: e.dma_start(out=GG[:], in_=sap(glag_h, 0, [[0, 128], [1, 128]])), writes=[r_gg])
                cOF = [sb("cOF%d" % i, [128, 4, 512], F32) for i in range(2)]
                cOB = [sb("cOB%d" % i, [128, 4, 512], F32) for i in range(2)]
                cGR = [sb("cGR%d" % i, [128, 4, 512], BF16) for i in range(2)]
                cGL = [sb("cGL%d" % i, [128, 4, 512], F32) for i in range(2)]
                cSQ = [sb("cSQ%d" % i, [128, 512], F32) for i in range(2)]
                cSS = [sb("cSS%d" % i, [128, 4], F32) for i in range(2)]
                r_cin = [Res(), Res()]
                r_cgl = [Res(), Res()]
                r_csq = [Res(), Res()]
                r_css = [Res(), Res()]

                def load_c(g):
                    b = g % 2
                    rows = lambda dram: dram.ap()[g * 512:(g + 1) * 512, :].rearrange("(t p) c -> p t c", p=128)
                    P.dma(lambda e: e.dma_start(out=cOF[b][:], in_=rows(OFD)), reads=[R_["OFD"]], writes=[r_cin[b]])
                    P.dma(lambda e: e.dma_start(out=cOB[b][:], in_=rows(OBD)), reads=[R_["OBD"]], pwrites=[r_cin[b]])
                    P.dma(lambda e: e.dma_start(out=cGR[b][:], in_=rows(GRD)), reads=[R_["GRD"]], pwrites=[r_cin[b]])

                def comb_tile(g, t):
                    b = g % 2
                    j = t % 2
                    ow = cGL[b][:, t, :]
                    o3 = sap(cGL[b], t * 512, [[2048, 128], [128, 4], [1, 128]])
                    SQo, SSc = cSQ[j], cSS[j]
                    P.dve(lambda e: e.tensor_tensor(ow, cOF[b][:, t, :], cOB[b][:, t, :], ALU.add), reads=[r_cin[b]], writes=[r_cgl[b]] if t == 0 else (), pwrites=() if t == 0 else [r_cgl[b]])
                    yield
                    P.act(lambda e: e.activation(SQo[:], ow, AF.Square), reads=[r_cgl[b]], writes=[r_csq[j]])
                    yield
                    P.dve(lambda e: e.reduce_sum(SSc[:], sap(SQo, 0, [[512, 128], [128, 4], [1, 128]]), AX.X), reads=[r_csq[j]], writes=[r_css[j]])
                    yield
                    P.dve(lambda e: e.tensor_scalar(SSc[:], SSc[:], 1.0 / 128, EPS, ALU.mult, ALU.add), reads=[r_css[j]], writes=[r_css[j]])
                    yield
                    P.act(lambda e: e.activation(SSc[:], SSc[:], AF.Ln), reads=[r_css[j]], writes=[r_css[j]])
                    yield
                    P.act(lambda e: e.activation(SSc[:], SSc[:], AF.Exp, scale=-0.5), reads=[r_css[j]], writes=[r_css[j]])
                    yield
                    P.dve(lambda e: e.tensor_tensor(o3, o3, sap(SSc, 0, [[4, 128], [1, 4], [0, 128]]), ALU.mult), reads=[r_cgl[b], r_css[j]], pwrites=[r_cgl[b]])
                    yield
                    P.dve(lambda e: e.tensor_tensor(o3, o3, sap(GG, 0, [[128, 128], [0, 4], [1, 128]]), ALU.mult), reads=[r_cgl[b], r_gg], pwrites=[r_cgl[b]])
                    yield
                    P.dve(lambda e: e.tensor_tensor(ow, ow, cGR[b][:, t, :], ALU.mult), reads=[r_cgl[b], r_cin[b]], pwrites=[r_cgl[b]])
                    yield


                def comb_all():
                    load_c(0)
                    yield
                    for g in range(8):
                        if g + 1 < 8:
                            load_c(g + 1)
                        for pr_ in ((0, 1), (2, 3)):
                            gs = [comb_tile(g, pr_[0]), comb_tile(g, pr_[1])]
                            while gs:
                                for x in list(gs):
                                    try:
                                        next(x)
                                    except StopIteration:
                                        gs.remove(x)
                                yield
                        P.dma(lambda e, g=g: e.dma_start(out=GLD.ap()[g * 512:(g + 1) * 512, :].rearrange("(t p) c -> p t c", p=128), in_=cGL[g % 2][:]),
                              reads=[r_cgl[g % 2]], pwrites=[R_["GLD"]])
                        yield

                comb_gen = comb_all()

                def comb_step(n):
                    for _ in range(n):
                        try:
                            next(comb_gen)
                        except StopIteration:
                            return False
                    return True


                P.dma(lambda e: e.dma_start(out=VALL[:], in_=VD.ap().rearrange("(n p) c -> p n c", p=128)), reads=[R_["VD"]], writes=[r_vall])
                for i in range(2):
                    P.dve(lambda e, i=i: e.memset(KTh[i][96:128, :], 0.0), writes=[r_kq[i]])
                    P.dve(lambda e, i=i: e.memset(QTh[i][96:128, :], 0.0), pwrites=[r_kq[i]])
                    P.dve(lambda e, i=i: e.memset(VA[i][:, :, 64:128], 1.0), writes=[r_va[i]])

                def load_head(h):
                    b = h % 2
                    P.dma(lambda e: e.dma_start(out=KTh[b][0:64, :], in_=KNT.ap()[h // 2, (h % 2) * 64:(h % 2) * 64 + 64, :]), reads=[R_["KNT"]], pwrites=[r_kq[b]])
                    P.dma(lambda e: e.dma_start(out=KTh[b][64:96, :], in_=KRT.ap()), reads=[R_["KRT"]], pwrites=[r_kq[b]])
                    P.dma(lambda e: e.dma_start(out=QTh[b][0:96, :], in_=QTD.ap()[h, :, :]), reads=[R_["QTD"]], pwrites=[r_kq[b]])
                    P.dve(lambda e: e.tensor_copy(VA[b][:, :, 0:64], VALL[:, :, h * 64:(h + 1) * 64]), reads=[r_vall], pwrites=[r_va[b]])

                def attend(h, qb):
                    b = h % 2
                    q0 = qb * 512
                    pot, poo, por = psO.next()
                    sc = {}

                    def S_(kt):
                        pt, po, pr = psS.next()
                        sc[kt] = (pt, po, pr)
                        P.pe(lambda e: e.matmul(pt[:, po:po + 512], KTh[b][:, kt * 128:(kt + 1) * 128], QTh[b][:, q0:q0 + 512], start=True, stop=True),
                             reads=[r_kq[b]], writes=[pr])

                    S_(0)
                    S_(1)
                    for kt in range(NT):
                        pt, po, pr = sc.pop(kt)
                        pbuf, rp = PTs.next()
                        P.act(lambda e, pt=pt, po=po, pbuf=pbuf: e.activation(pbuf[:], pt[:, po:po + 512], AF.Exp), reads=[pr], writes=[rp])
                        if kt + 2 < NT:
                            S_(kt + 2)
                        P.pe(lambda e, kt=kt, pbuf=pbuf: e.matmul(pot[:, poo:poo + 512], VA[b][:, kt, :], pbuf[:], start=(kt == 0), stop=(kt == NT - 1)),
                             reads=[r_va[b], rp], writes=[por] if kt == 0 else (), pwrites=() if kt == 0 else [por])
                    den, rden = DEN.next()
                    ot, rot = OTs.next()
                    P.dve(lambda e: e.reciprocal(den[64:128, :], pot[64:128, poo:poo + 512]), reads=[por], writes=[rden])
                    P.dve(lambda e: e.tensor_tensor(ot[0:64, :], pot[0:64, poo:poo + 512], den[64:128, :], ALU.mult), reads=[por, rden], writes=[rot])
                    P.dma(lambda e: e.dma_start(out=MTD.ap()[h * 64:(h + 1) * 64, q0:q0 + 512], in_=ot[0:64, :]), reads=[rot], pwrites=[R_["MTD"]])

                load_head(0)
                for h in range(min(8, DEV_AH)):
                    if h + 1 < 8:
                        load_head(h + 1)
                    for qb in range(min(8, DEV_AQ)):
                        comb_step(3)
                        attend(h, qb)
                while comb_step(8):
                    pass
                P.barrier()

        P.default_q = "sp"

        def ln_core(sbt, T1, rT1, MV, rMV, XN, rXN, dst=None, rdst=None, wdst=None, gt=None, bt=None, rgb=None):
            XNa = XN if isinstance(XN, bass.AP) else XN[:]
            P.act(lambda e: e.activation(XNa, T1[:], AF.Square), reads=[rT1], writes=[rXN])
            P.dve(lambda e: e.reduce_sum(MV[:, 0:1], T1[:], AX.X), reads=[rT1], writes=[rMV])
            yield
            P.dve(lambda e: e.reduce_sum(MV[:, 1:2], XNa, AX.X), reads=[rXN], pwrites=[rMV])
            yield
            P.dve(lambda e: e.tensor_scalar_mul(MV[:, 0:2], MV[:, 0:2], 1.0 / D), reads=[rMV], writes=[rMV])
            yield
            P.dve(lambda e: e.tensor_tensor(MV[:, 2:3], MV[:, 0:1], MV[:, 0:1], ALU.mult), reads=[rMV], writes=[rMV])
            yield
            P.dve(lambda e: e.scalar_tensor_tensor(MV[:, 3:4], MV[:, 1:2], EPS, MV[:, 2:3], ALU.add, ALU.subtract), reads=[rMV], writes=[rMV])
            yield
            P.act(lambda e: e.activation(MV[:, 3:4], MV[:, 3:4], AF.Ln), reads=[rMV], writes=[rMV])
            yield
            P.act(lambda e: e.activation(MV[:, 3:4], MV[:, 3:4], AF.Exp, scale=-0.5), reads=[rMV], writes=[rMV])
            yield
            P.dve(lambda e: e.scalar_tensor_tensor(dst, T1[:], MV[:, 0:1], gt[:], ALU.subtract, ALU.mult), reads=[rT1, rMV, rgb], **wdst)
            yield
            P.dve(lambda e: e.scalar_tensor_tensor(dst, dst, MV[:, 3:4], bt[:], ALU.mult, ALU.add), reads=[rdst, rMV, rgb], writes=[rdst])
            yield

        if upto >= 4:
            with ExitStack() as st4:
                HT = st4.enter_context(nc.sbuf_tensor("HT", [128, 8, S], BF16))
                COMBT = st4.enter_context(nc.sbuf_tensor("COMBT", [128, S], BF16))
                r_ht, r_combt = Res(), Res()
                P.dve(lambda e: e.memset(COMBT[:], 0.0), writes=[r_combt])
                with ExitStack() as st:
                    def sb(name, shape, dt):
                        return st.enter_context(nc.sbuf_tensor(name, list(shape), dt))

                    WO = sb("WO", [128, 8, D], BF16)
                    LG1 = sb("LG1", [128, D], F32)
                    LB1 = sb("LB1", [128, D], F32)
                    WR = sb("WR", [128, 8, 20], F32)
                    BR = sb("BR", [128, 20], F32)
                    r_c = Res()
                    XTs = [sb("XT%d" % i, [128, 2, D], F32) for i in range(2)]
                    GLt = [sb("GLt%d" % i, [128, 2, 512], F32) for i in range(2)]
                    MIX = [sb("MIX%d" % i, [128, 8, 256], BF16) for i in range(2)]
                    r_xt, r_glt, r_mix = [Res(), Res()], [Res(), Res()], [Res(), Res()]
                    T1s = [sb("T1_%d" % i, [128, D], F32) for i in range(2)]
                    XN = sb("XN", [128, D], F32)
                    X1 = [sb("X1_%d" % i, [128, 2, D], F32) for i in range(2)]
                    HHs = [sb("HH%d" % i, [128, D], F32) for i in range(2)]
                    HTfs = [sb("HTf%d" % i, [128, 8, 128], F32) for i in range(2)]
                    MVs = [sb("MV%d" % i, [128, 8], F32) for i in range(2)]
                    RTs = [sb("RT%d" % i, [128, 96], F32) for i in range(2)]
                    r_t1s, r_xn, r_x1, r_hhs, r_htfs, r_mvs, r_rts = [Res(), Res()], Res(), [[Res(), Res()], [Res(), Res()]], [Res(), Res()], [Res(), Res()], [Res(), Res()], [Res(), Res()]
                    r_ps3 = Res()
                    r_b7 = [[r_ps3, r_ps3], [r_ps3, r_ps3]]
                    wo_v = wo_h.ap().rearrange("(k p) n -> p k n", p=128)
                    for k in range(8):
                        ws, rws = XTs[k % 2], r_xt[k % 2]
                        P.dma(lambda e, k=k, ws=ws: e.dma_start(out=ws[:, 0, :], in_=wo_v[:, k, :]), writes=[rws])
                        if k % 2 == 0:
                            P.dve(lambda e, k=k, ws=ws: e.tensor_copy(WO[:, k, :], ws[:, 0, :]), reads=[rws], pwrites=[r_c])
                        else:
                            P.act(lambda e, k=k, ws=ws: e.copy(WO[:, k, :], ws[:, 0, :]), reads=[rws], pwrites=[r_c])
                    P.dma(lambda e: e.dma_start(out=LG1[:], in_=sap(ln1g_h, 0, [[0, 128], [1, D]])), pwrites=[r_c])
                    P.dma(lambda e: e.dma_start(out=LB1[:], in_=sap(ln1b_h, 0, [[0, 128], [1, D]])), pwrites=[r_c])
                    P.dma(lambda e: e.dma_start(out=WR[:], in_=wr_h.ap().rearrange("(k p) n -> p k n", p=128)), pwrites=[r_c])
                    P.dma(lambda e: e.dma_start(out=BR[:], in_=sap(br_h, 0, [[0, 128], [1, 20]])), pwrites=[r_c])

                    def load_a(g):
                        b = g % 2
                        s0 = g * 256
                        P.dma(lambda e: e.dma_start(out=XTs[b][:], in_=x_h.ap()[s0:s0 + 256, :].rearrange("(t p) d -> p t d", p=128)), writes=[r_xt[b]])
                        P.dma(lambda e: e.dma_start(out=GLt[b][:], in_=GLD.ap()[s0:s0 + 256, :].rearrange("(t p) d -> p t d", p=128)), reads=[R_["GLD"]], writes=[r_glt[b]])
                        P.dma(lambda e: e.dma_start(out=MIX[b][:, 4:8, :], in_=MTD.ap().rearrange("(k p) t -> p k t", p=128)[:, :, s0:s0 + 256]),
                              reads=[R_["MTD"]], writes=[r_mix[b]])

                    def tile_a(tt):
                        b = (tt // 2) % 2
                        tl = tt % 2
                        s0 = tt * 128
                        x1h, rx1 = X1[b], r_x1[b][tl]
                        x1 = x1h[:, tl, :]
                        xt_t = XTs[b][:, tl, :]
                        T1, r_t1 = T1s[tl], r_t1s[tl]
                        HH, r_hh = HHs[tl], r_hhs[tl]
                        HTf, r_htf = HTfs[tl], r_htfs[tl]
                        MV, r_mv = MVs[tl], r_mvs[tl]
                        RT, r_rt = RTs[tl], r_rts[tl]
                        pt, po, pr = bank(6)
                        pr = r_ps3
                        for k in range(4):
                            P.pe(lambda e, k=k: e.transpose(pt[:, po + k * 128:po + (k + 1) * 128], GLt[b][:, tl, k * 128:(k + 1) * 128], ident_f[:]),
                                 reads=[r_glt[b], r_ident], writes=[pr] if k == 0 else (), pwrites=() if k == 0 else [pr])
                        P.act(lambda e: e.copy(sap(MIX[b], tl * 128, [[2048, 128], [256, 4], [1, 128]]), sap(pt, po, [[1024, 128], [128, 4], [1, 128]])), reads=[pr], pwrites=[r_mix[b]])
                        yield
                        PY, rpy = PSt[tl], PSr[tl][0]
                        for nh in range(2):
                            for k in range(8):
                                first = (nh == 0 and k == 0)
                                P.pe(lambda e, nh=nh, k=k: e.matmul(PY[:, nh * 512:(nh + 1) * 512], MIX[b][:, k, tl * 128:(tl + 1) * 128], WO[:, k, nh * 512:(nh + 1) * 512], start=(k == 0), stop=(k == 7)),
                                     reads=[r_mix[b], r_c], writes=[rpy] if first else (), pwrites=() if first else [rpy])
                        yield
                        P.dve(lambda e: e.tensor_tensor(T1[:], PY[:], MODL[:, 2048:3072], ALU.mult), reads=[rpy, r_MODL], writes=[r_t1])
                        yield
                        P.dve(lambda e: e.scalar_tensor_tensor(T1[:], xt_t, ALPHA, T1[:], ALU.mult, ALU.add), reads=[r_xt[b], r_t1], writes=[r_t1])
                        yield
                        yield from ln_core(None, T1, r_t1, MV, r_mv, x1, rx1, dst=x1, rdst=rx1, wdst=dict(writes=[rx1]), gt=LG1, bt=LB1, rgb=r_c)
                        if tl == 1:
                            g0 = (tt // 2) * 256
                            P.dma(lambda e: e.dma_start(out=X1D.ap()[g0:g0 + 256, :].rearrange("(t p) d -> p t d", p=128), in_=x1h[:]), reads=r_x1[b], pwrites=[R_["X1D"]])
                        P.dve(lambda e: e.tensor_tensor(HH[:], x1, MODL[:, 4096:5120], ALU.mult), reads=[rx1, r_MODL], writes=[r_hh])
                        yield
                        P.dve(lambda e: e.tensor_tensor(HH[:], HH[:], MODL[:, 3072:4096], ALU.add), reads=[r_hh, r_MODL], writes=[r_hh])
                        yield
                        PH, rph = PSt[2], PSr[2][0]
                        for k in range(8):
                            P.pe(lambda e, k=k: e.transpose(PH[:, k * 128:(k + 1) * 128], HH[:, k * 128:(k + 1) * 128], ident_f[:]),
                                 reads=[r_hh, r_ident], writes=[rph] if k == 0 else (), pwrites=() if k == 0 else [rph])
                        P.act(lambda e: e.copy(sap(HTf, 0, [[1024, 128], [1, 1024]]), PH[:]), reads=[rph], writes=[r_htf])
                        yield
                        P.act(lambda e: e.copy(HT[:, :, s0:s0 + 128], HTf[:]), reads=[r_htf], pwrites=[r_ht])
                        pt2, po2_, _ = bank(7)
                        po2 = po2_ + tl * 256
                        pr2, pr3 = r_b7[tl]
                        for k in range(8):
                            P.pe(lambda e, k=k: e.matmul(pt2[:, po2:po2 + 20], HTf[:, k, :], WR[:, k, :], start=(k == 0), stop=(k == 7)),
                                 reads=[r_htf, r_c], writes=[pr2] if k == 0 else (), pwrites=() if k == 0 else [pr2])
                        yield
                        LOG = RT[:, 0:20]; GMX = RT[:, 20:21]; G1H = RT[:, 24:28]; GE = RT[:, 28:32]; PG_ = RT[:, 32:33]
                        MSK = RT[:, 36:52]; EIN = RT[:, 52:56]; M1 = RT[:, 56:57]; MK1 = RT[:, 60:64]; E2 = RT[:, 64:68]
                        M2 = RT[:, 57:58]; MK2 = RT[:, 68:72]; RR = RT[:, 58:59]; W1 = RT[:, 59:60]; W2 = RT[:, 72:73]; CW = RT[:, 76:80]
                        CMB = RT[:, 80:96]
                        rr_ = dict(reads=[r_rt], writes=[r_rt])
                        P.dve(lambda e: e.tensor_tensor(LOG, pt2[:, po2:po2 + 20], BR[:], ALU.add), reads=[pr2, r_c], writes=[r_rt])
                        yield
                        P.dve(lambda e: e.reduce_max(GMX, RT[:, 0:4], AX.X), **rr_)
                        yield
                        P.dve(lambda e: e.tensor_scalar(G1H, RT[:, 0:4], GMX, None, ALU.is_equal), **rr_)
                        P.dve(lambda e: e.tensor_scalar(GE, RT[:, 0:4], GMX, None, ALU.subtract), **rr_)
                        yield
                        P.act(lambda e: e.activation(GE, GE, AF.Exp), **rr_)
                        yield
                        P.dve(lambda e: e.reduce_sum(PG_, GE, AX.X), **rr_)
                        yield
                        P.dve(lambda e: e.reciprocal(PG_, PG_), **rr_)
                        yield
                        P.dve(lambda e: e.tensor_tensor(sap(RT, 36, [[96, 128], [4, 4], [1, 4]]), sap(RT, 4, [[96, 128], [4, 4], [1, 4]]),
                                                        sap(RT, 24, [[96, 128], [1, 4], [0, 4]]), ALU.mult), **rr_)
                        yield
                        P.dve(lambda e: e.reduce_sum(EIN, sap(RT, 36, [[96, 128], [1, 4], [4, 4]]), AX.X), **rr_)
                        yield
                        P.dve(lambda e: e.reduce_max(M1, EIN, AX.X), **rr_)
                        yield
                        P.dve(lambda e: e.tensor_scalar(MK1, EIN, M1, None, ALU.is_equal), **rr_)
                        yield
                        P.dve(lambda e: e.scalar_tensor_tensor(E2, MK1, -1e30, EIN, ALU.mult, ALU.add), **rr_)
                        yield
                        P.dve(lambda e: e.reduce_max(M2, E2, AX.X), **rr_)
                        yield
                        P.dve(lambda e: e.tensor_scalar(MK2, E2, M2, None, ALU.is_equal), **rr_)
                        P.dve(lambda e: e.tensor_tensor(RR, M2, M1, ALU.subtract), **rr_)
                        yield
                        P.act(lambda e: e.activation(RR, RR, AF.Exp), **rr_)
                        yield
                        P.dve(lambda e: e.tensor_scalar_add(W1, RR, 1.0), **rr_)
                        yield
                        P.dve(lambda e: e.reciprocal(W1, W1), **rr_)
                        yield
                        P.dve(lambda e: e.tensor_tensor(W1, W1, PG_, ALU.mult), **rr_)
                        yield
                        P.dve(lambda e: e.tensor_tensor(W2, W1, RR, ALU.mult), **rr_)
                        P.dve(lambda e: e.tensor_scalar(CW, MK1, W1, None, ALU.mult), **rr_)
                        yield
                        P.dve(lambda e: e.scalar_tensor_tensor(CW, MK2, W2, CW, ALU.mult, ALU.add), **rr_)
                        yield
                        P.dve(lambda e: e.tensor_tensor(sap(RT, 80, [[96, 128], [4, 4], [1, 4]]), sap(RT, 24, [[96, 128], [1, 4], [0, 4]]),
                                                        sap(RT, 76, [[96, 128], [0, 4], [1, 4]]), ALU.mult), **rr_)
                        yield
                        po3 = po2_ + 128 + tl * 256
                        P.pe(lambda e: e.transpose(pt2[0:16, po3:po3 + 128], CMB, ident_f[:]), reads=[r_rt, r_ident], writes=[pr3])
                        P.dve(lambda e: e.tensor_copy(COMBT[0:16, s0:s0 + 128], pt2[0:16, po3:po3 + 128]), reads=[pr3], pwrites=[r_combt])
                        if debug and DEV_DBGC:
                            P.dma(lambda e: e.dma_start(out=dbg_comb.ap()[s0:s0 + 128, :], in_=CMB), reads=[r_rt])
                        yield

                    if debug:
                        dbg_comb = nc.dram_tensor("dbg_comb", [S, 16], F32, kind="ExternalOutput")
                    load_a(0)
                    for g in range(16):
                        if g + 1 < 16:
                            load_a(g + 1)
                        drive([tile_a(2 * g), tile_a(2 * g + 1)])
                    P.barrier()

                if upto >= 5:
                    with ExitStack() as st:
                        def sb(name, shape, dt):
                            return st.enter_context(nc.sbuf_tensor(name, list(shape), dt))

                        SELi = sb("SELi", [128, 16, 128], I32)
                        SELf = sb("SELf", [128, 16, 128], F32)
                        SEL = sb("SEL", [128, 16, 128], BF16)
                        r_sel = Res()
                        P.pool(lambda e: e.iota(SELi[:], pattern=[[1, 16], [0, 128]], base=0, channel_multiplier=-1), writes=[r_sel])
                        P.dve(lambda e: e.tensor_copy(SELf[:], SELi[:]), reads=[r_sel], writes=[r_sel])
                        P.dve(lambda e: e.tensor_single_scalar(SEL[:], SELf[:], 0.0, ALU.is_equal), reads=[r_sel], writes=[r_sel])
                        WST = Rot([(sb("WS4_%d" % i, [128, 8, 256], F32), Res()) for i in range(2)])
                        WG = [sb("WG%d" % i, [128, 8, 256], BF16) for i in range(2)]
                        WU = [sb("WU%d" % i, [128, 8, 256], BF16) for i in range(2)]
                        r_wg = [Res(), Res()]
                        SG = Rot([(sb("SG%d" % i, [128, 512], BF16), Res()) for i in range(2)])
                        TT = Rot([(sb("TT%d" % i, [128, 512], BF16), Res()) for i in range(2)])
                        HEs = Rot([(sb("HEs%d" % i, [128, 2, 2048], BF16), Res()) for i in range(DEV_NHE)])
                        he_cur = [None]
                        psG = Rot([bank(0), bank(1)])
                        psU = Rot([bank(2), bank(3)])
                        psC = Rot([bank(4), bank(5)])

                        def load_w(ex):
                            b = ex % 2
                            for j, (wh, dst) in enumerate(((weg_h, WG[b]), (weu_h, WU[b]))):
                                ws, rws = WST.next()
                                P.dma(lambda e, ws=ws, wh=wh: e.dma_start(out=ws[:], in_=wh.ap()[ex].rearrange("(k p) f -> p k f", p=128)), writes=[rws])
                                wr = dict(writes=[r_wg[b]]) if j == 0 else dict(pwrites=[r_wg[b]])
                                if j == 0:
                                    P.dve(lambda e, ws=ws, dst=dst: e.tensor_copy(dst[:], ws[:]), reads=[rws], **wr)
                                else:
                                    P.act(lambda e, ws=ws, dst=dst: e.copy(dst[:], ws[:]), reads=[rws], **wr)

                        def expert_blk(ex, blk):
                            b = ex % 2
                            c0 = blk * 512
                            pct, pco, pcr = psC.next()
                            P.pe(lambda e: e.matmul(pct[:, pco:pco + 512], SEL[:, ex, :], COMBT[:, c0:c0 + 512], start=True, stop=True),
                                 reads=[r_sel, r_combt], writes=[pcr])
                            if blk % 4 == 0:
                                he_cur[0] = HEs.next()
                            he, rhe = he_cur[0]
                            hc = (blk % 4) * 512
                            for mc in range(2):
                                pgt, pgo, pgr = psG.next()
                                put, puo, pur = psU.next()
                                for k in range(8):
                                    P.pe(lambda e, k=k, mc=mc, pgt=pgt, pgo=pgo: e.matmul(pgt[:, pgo:pgo + 512], WG[b][:, k, mc * 128:(mc + 1) * 128], HT[:, k, c0:c0 + 512], start=(k == 0), stop=(k == 7)),
                                         reads=[r_wg[b], r_ht], writes=[pgr] if k == 0 else (), pwrites=() if k == 0 else [pgr])
                                for k in range(8):
                                    P.pe(lambda e, k=k, mc=mc, put=put, puo=puo: e.matmul(put[:, puo:puo + 512], WU[b][:, k, mc * 128:(mc + 1) * 128], HT[:, k, c0:c0 + 512], start=(k == 0), stop=(k == 7)),
                                         reads=[r_wg[b], r_ht], writes=[pur] if k == 0 else (), pwrites=() if k == 0 else [pur])
                                if DEV_B < 3:
                                    continue
                                sg, rsg = SG.next()
                                tt_, rtt = TT.next()
                                P.act(lambda e, sg=sg, pgt=pgt, pgo=pgo: e.activation(sg[:], pgt[:, pgo:pgo + 512], AF.Silu), reads=[pgr], writes=[rsg])
                                P.dve(lambda e, sg=sg, tt_=tt_, put=put, puo=puo: e.tensor_tensor(tt_[:], put[:, puo:puo + 512], sg[:], ALU.mult), reads=[pur, rsg], writes=[rtt])
                                first = (mc == 0 and blk % 4 == 0)
                                P.dve(lambda e, tt_=tt_, mc=mc: e.tensor_tensor(he[:, mc, hc:hc + 512], pct[:, pco:pco + 512], tt_[:], ALU.mult), reads=[pcr, rtt],
                                      writes=[rhe] if first else (), pwrites=() if first else [rhe])
                            if DEV_B >= 4 and blk % 4 == 3:
                                g0 = (blk // 4) * 2048
                                P.dma(lambda e: e.dma_start(out=HED.ap()[ex if DEV_HEDX else 0].rearrange("(m p) t -> p m t", p=128)[:, :, g0:g0 + 2048], in_=he[:]), reads=[rhe], pwrites=[R_["HED"]], q=DEV_HQ)

                        load_w(0)
                        for ex in range(min(NE, DEV_BE)):
                            if ex + 1 < NE:
                                load_w(ex + 1)
                            for blk in range(8 if DEV_B >= 2 else 0):
                                expert_blk(ex, blk)
                        P.barrier()
            P.barrier()
        if upto >= 6:
            with ExitStack() as st:
                def sb(name, shape, dt):
                    return st.enter_context(nc.sbuf_tensor(name, list(shape), dt))

                WD = sb("WD", [128, 32, D], BF16)
                LG2 = sb("LG2", [128, D], F32)
                LB2 = sb("LB2", [128, D], F32)
                r_cc = Res()
                HEb = [sb("HEb%d" % i, [128, 32, 512], BF16) for i in range(2)]
                r_heb = [Res(), Res()]
                X1t = [sb("X1t%d" % i, [128, 2, D], F32) for i in range(2)]
                r_x1t = [Res(), Res()]
                T1c = sb("T1c", [128, D], F32); XNc = sb("XNc", [128, D], F32)
                OUTt = [sb("OUTt%d" % i, [128, 2, D], F32) for i in range(2)]
                r_out = [Res(), Res()]
                MVc = sb("MVc", [128, 8], F32)
                r_t1c, r_xnc, r_mvc = Res(), Res(), Res()
                wd_v = wed_h.ap().rearrange("e (m p) n -> p (e m) n", p=128)
                r_wd = [Res() for _ in range(16)]
                for e2 in range(16):
                    ws, rws = X1t[e2 % 2], r_x1t[e2 % 2]
                    P.dma(lambda e, e2=e2, ws=ws: e.dma_start(out=ws[:], in_=wd_v[:, 2 * e2:2 * e2 + 2, :]), writes=[rws])
                    if e2 % 2 == 0:
                        P.dve(lambda e, e2=e2, ws=ws: e.tensor_copy(WD[:, 2 * e2:2 * e2 + 2, :], ws[:]), reads=[rws], writes=[r_wd[e2]])
                    else:
                        P.act(lambda e, e2=e2, ws=ws: e.copy(WD[:, 2 * e2:2 * e2 + 2, :], ws[:]), reads=[rws], writes=[r_wd[e2]])
                P.dma(lambda e: e.dma_start(out=LG2[:], in_=sap(ln2g_h, 0, [[0, 128], [1, D]])), pwrites=[r_cc])
                P.dma(lambda e: e.dma_start(out=LB2[:], in_=sap(ln2b_h, 0, [[0, 128], [1, D]])), pwrites=[r_cc])
                hed_v = HED.ap().rearrange("e (m p) t -> p (e m) t", p=128)

                def load_he(blk):
                    b = blk % 2
                    c0 = blk * 512
                    P.dma(lambda e: e.dma_start(out=HEb[b][:], in_=hed_v[:, :, c0:c0 + 512]), reads=[R_["HED"]], writes=[r_heb[b]])

                def load_x1(g):
                    b = g % 2
                    P.dma(lambda e: e.dma_start(out=X1t[b][:], in_=X1D.ap()[g * 256:(g + 1) * 256, :].rearrange("(t p) d -> p t d", p=128)), reads=[R_["X1D"]], writes=[r_x1t[b]])

                def tile_c(tt):
                    blk, t = tt // 4, tt % 4
                    hb, rhb = HEb[blk % 2], r_heb[blk % 2]
                    b = (tt // 2) % 2
                    tl = tt % 2
                    PY, rpy = PSt[tt % 2], PSr[tt % 2][0]
                    for nh in range(2):
                        for em in range(32):
                            first = (nh == 0 and em == 0)
                            P.pe(lambda e, nh=nh, em=em: e.matmul(PY[:, nh * 512:(nh + 1) * 512], hb[:, em, t * 128:(t + 1) * 128], WD[:, em, nh * 512:(nh + 1) * 512], start=(em == 0), stop=(em == 31)),
                                 reads=[rhb, r_wd[em // 2]], writes=[rpy] if first else (), pwrites=() if first else [rpy])
                    oth, rot = OUTt[b], r_out[b]
                    ot = oth[:, tl, :]
                    P.dve(lambda e: e.tensor_tensor(T1c[:], PY[:], MODL[:, 5120:6144], ALU.mult), reads=[rpy, r_MODL], writes=[r_t1c])
                    P.dve(lambda e: e.scalar_tensor_tensor(T1c[:], X1t[b][:, tl, :], ALPHA, T1c[:], ALU.mult, ALU.add), reads=[r_x1t[b], r_t1c], writes=[r_t1c])
                    wo_ = dict(writes=[rot]) if tl == 0 else dict(pwrites=[rot])
                    drive([ln_core(None, T1c, r_t1c, MVc, r_mvc, XNc, r_xnc, dst=ot, rdst=rot, wdst=wo_, gt=LG2, bt=LB2, rgb=r_cc)])
                    if tl == 1:
                        g0 = (tt // 2) * 256
                        P.dma(lambda e: e.dma_start(out=out_h.ap()[g0:g0 + 256, :].rearrange("(t p) d -> p t d", p=128), in_=oth[:]), reads=[rot], pwrites=[R_["OUT"]])

                load_he(0)
                load_x1(0)
                for tt in range(32):
                    if tt % 4 == 0 and tt // 4 + 1 < 8:
                        load_he(tt // 4 + 1)
                    if tt % 2 == 0 and tt // 2 + 1 < 16:
                        load_x1(tt // 2 + 1)
                    tile_c(tt)
                P.barrier()

        P.barrier()
        finals = [op for op in P.all if op.dma]
        P.emit(final_ops=finals)
    return nc, P


def make_in_maps(inputs):
    f = lambda a: np.ascontiguousarray(np.asarray(a, dtype=np.float32))
    x = f(inputs["x"]); c = f(inputs["c"]); ctx = f(inputs["ctx"])
    wgk = np.zeros((33, 512), np.float32)
    wgk[0:16, 0:256] = inputs["w_gk_f"][0]
    wgk[16:32, 256:512] = inputs["w_gk_b"][0]
    wgk[32, 0:256] = inputs["b_gk_f"][0]
    wgk[32, 256:512] = inputs["b_gk_b"][0]
    shared = {
        "c_ctx": f(inputs["c_ctx"]),
        "w_ada": f(inputs["w_ada"][0]), "b_ada": f(inputs["b_ada"][0]),
        "w_in": f(inputs["w_in"][0]), "wgk": wgk,
        "gla_g": f(inputs["gla_norm_g"][0]), "q_g": f(inputs["mla_q_norm_g"][0]),
        "w_uq": f(inputs["w_uq"][0]), "kv_g": f(inputs["mla_kv_norm_g"][0]),
        "w_ukv": f(inputs["w_ukv"][0]), "w_o": f(inputs["w_o"][0]),
        "ln1_g": f(inputs["ln1_g"][0]), "ln1_b": f(inputs["ln1_b"][0]),
        "ln2_g": f(inputs["ln2_g"][0]), "ln2_b": f(inputs["ln2_b"][0]),
        "w_r": f(np.concatenate([inputs["w_router_group"][0], inputs["w_router_expert"][0]], axis=1)),
        "b_r": f(np.concatenate([inputs["b_router_group"][0], inputs["b_router_expert"][0]], axis=0)),
        "w_eg": f(inputs["w_expert_gate"][0]), "w_eu": f(inputs["w_expert_up"][0]),
        "w_ed": f(inputs["w_expert_down"][0]),
    }
    maps = []
    for b in range(8):
        m = dict(shared)
        m["x"] = x[b]
        m["ctx"] = ctx[b]
        m["c"] = c[b]
        maps.append(m)
    return maps


_CACHE = {}


def kernel(**inputs):
    if "nc" not in _CACHE:
        _CACHE["nc"] = build()[0]
    nc = _CACHE["nc"]
    in_maps = make_in_maps(inputs)
    res = run_bass_kernel_spmd(nc, in_maps, core_ids=list(range(8)))
    return np.stack([np.asarray(r["out"]).reshape(S, D) for r in res.results], axis=0).astype(np.float32)
```
